# Optimizing a Trainium2 kernel written in Bass

```python
import math
import jax, jax.numpy as jnp
from jax import lax
import numpy as np

D_MODEL = 1024
BATCH = 2
SEQ = 8192
DEPTH = 1

DN_HEADS = 8
DN_HEAD_DIM = 128
DN_WIDTH = DN_HEADS * DN_HEAD_DIM
DN_CONV = 5
CHUNK = 64
SC_WIDTH = D_MODEL
SC_CONV = 3
N_BRANCH = 2
IN_SIZES = (3 * DN_WIDTH, DN_WIDTH, 2 * DN_HEADS, 2 * DN_HEADS, 3 * SC_WIDTH, N_BRANCH * D_MODEL)
IN_WIDTH = sum(IN_SIZES)
N_EXPERTS = 32
TOP_K = 4
D_FF = D_MODEL
SWIGLU_LIMIT = 7.0
SWIGLU_ALPHA = 1.702
MOE_BLOCK = 256
EPS = 1e-6

kernel_name = "hybrid_gdn_shortconv_moe_adaln_encoder"


def rmsnorm(x, w):
    xf = x.astype(jnp.float32)
    xf = xf * lax.rsqrt(jnp.mean(xf * xf, axis=-1, keepdims=True) + EPS)
    return (xf * w.astype(jnp.float32)).astype(x.dtype)


def l2norm(x):
    return x * lax.rsqrt(jnp.sum(x * x, axis=-1, keepdims=True) + EPS)


def dwconv_centred(x, w):
    width, ch = w.shape
    pad = width // 2
    return lax.conv_general_dilated(
        x, w[:, None, :].astype(x.dtype), window_strides=(1,), padding=[(pad, pad)],
        dimension_numbers=("NWC", "WIO", "NWC"), feature_group_count=ch)


def unit_lower_inverse(L):
    c = L.shape[-1]
    n = -L
    p = jnp.eye(c, dtype=L.dtype) + n
    npow = n
    for _ in range(int(math.log2(c)) - 1):
        npow = npow @ npow
        p = p + p @ npow
    return p


def gated_delta_rule(q, k, v, beta, g):
    bsz, nh, t, dk = q.shape
    dv = v.shape[-1]
    n = t // CHUNK
    q = q * (dk ** -0.5)
    ch = lambda a: a.reshape(bsz, nh, n, CHUNK, *a.shape[3:])
    q, k, v, beta, g = ch(q), ch(k), ch(v), ch(beta), ch(g)
    g = jnp.cumsum(g, axis=-1)
    idx = jnp.arange(CHUNK)
    incl = idx[:, None] >= idx[None, :]
    strict = idx[:, None] > idx[None, :]
    decay = jnp.exp(jnp.where(incl, g[..., :, None] - g[..., None, :], -jnp.inf))
    k_beta = k * beta[..., None]
    L = jnp.where(strict, jnp.einsum("bhnid,bhnjd->bhnij", k_beta, k) * decay, 0.0)
    t_inv = unit_lower_inverse(L)
    u = t_inv @ (v * beta[..., None])
    w = t_inv @ (k_beta * jnp.exp(g)[..., None])
    a_qk = jnp.einsum("bhnid,bhnjd->bhnij", q, k) * decay
    g_last = g[..., -1]
    k_dec = k * jnp.exp(g_last[..., None] - g)[..., None]
    q_dec = q * jnp.exp(g)[..., None]

    def step(state, xs):
        q_c, k_c, u_c, w_c, a_c, gl = xs
        v_new = u_c - w_c @ state
        o_c = q_c @ state + a_c @ v_new
        state = state * jnp.exp(gl)[..., None, None] + jnp.einsum("bhck,bhcv->bhkv", k_c, v_new)
        return state, o_c

    xs = tuple(jnp.moveaxis(a, 2, 0) for a in (q_dec, k_dec, u, w, a_qk, g_last))
    s0 = jnp.zeros((bsz, nh, dk, dv), jnp.float32)
    _, o = lax.scan(step, s0, xs)
    return jnp.moveaxis(o, 0, 2).reshape(bsz, nh, t, dv)


def hybrid_mixer(h, w_in, conv_qkv_w, a_log, dt_bias, onorm_w, w_up_a, conv_sc_w, w_out_sc, w_o):
    bsz, t, _ = h.shape
    p = h @ w_in
    cuts = np.cumsum(IN_SIZES)[:-1].tolist()
    qkv, z, b_raw, a_raw, sc, gates = jnp.split(p, cuts, axis=-1)

    qkv = jax.nn.silu(dwconv_centred(qkv, conv_qkv_w)).astype(jnp.float32)
    heads = lambda a: a.reshape(bsz, t, DN_HEADS, DN_HEAD_DIM).transpose(0, 2, 1, 3)
    q, k, v = (heads(a) for a in jnp.split(qkv, 3, axis=-1))
    q, k = l2norm(q), l2norm(k)
    beta = jax.nn.sigmoid(b_raw.astype(jnp.float32)).reshape(bsz, t, 2, DN_HEADS)
    g = -jnp.exp(a_log.astype(jnp.float32)) * jax.nn.softplus(
        a_raw.astype(jnp.float32).reshape(bsz, t, 2, DN_HEADS) + dt_bias.astype(jnp.float32))
    bh = lambda a, d: a[:, :, d].transpose(0, 2, 1)
    o_fwd = gated_delta_rule(q, k, v, bh(beta, 0), bh(g, 0))
    o_bwd = jnp.flip(gated_delta_rule(jnp.flip(q, 2), jnp.flip(k, 2), jnp.flip(v, 2),
                                      jnp.flip(bh(beta, 1), -1), jnp.flip(bh(g, 1), -1)), 2)
    o = (o_fwd + o_bwd).transpose(0, 2, 1, 3)
    o = o * lax.rsqrt(jnp.mean(o * o, axis=-1, keepdims=True) + EPS) * onorm_w.astype(jnp.float32)
    o = o.astype(h.dtype) * jax.nn.silu(z.reshape(bsz, t, DN_HEADS, DN_HEAD_DIM))
    y_a = o.reshape(bsz, t, DN_WIDTH) @ w_up_a

    b_gate, c_gate, u_in = jnp.split(sc, 3, axis=-1)
    y_b = (b_gate * dwconv_centred(c_gate * u_in, conv_sc_w)) @ w_out_sc

    g_a, g_b = jnp.split(gates, N_BRANCH, axis=-1)
    return (jax.nn.sigmoid(g_a) * y_a + jax.nn.sigmoid(g_b) * y_b) @ w_o


def moe_ffn(h, router_w, router_b, w1, b1, w2, b2):
    bsz, t, d = h.shape
    n_tok = bsz * t
    hf = h.reshape(n_tok, d)
    logits = (hf @ router_w + router_b).astype(jnp.float32)
    top_val, top_idx = lax.top_k(logits, TOP_K)
    gates = jax.nn.softmax(top_val, axis=-1)
    m = n_tok * TOP_K
    e_flat = top_idx.reshape(m)
    t_flat = jnp.repeat(jnp.arange(n_tok, dtype=jnp.int32), TOP_K)
    g_flat = gates.reshape(m)
    order = jnp.argsort(e_flat)
    e_s, t_s, g_s = e_flat[order], t_flat[order], g_flat[order]
    counts = jnp.bincount(e_flat, length=N_EXPERTS)
    padded = (counts + MOE_BLOCK - 1) // MOE_BLOCK * MOE_BLOCK
    start = jnp.cumsum(counts) - counts
    pad_end = jnp.cumsum(padded)
    pad_start = pad_end - padded
    dest = pad_start[e_s] + (jnp.arange(m) - start[e_s])
    n_blocks = -(-m // MOE_BLOCK) + N_EXPERTS
    m_pad = n_blocks * MOE_BLOCK
    tok_buf = jnp.full((m_pad,), n_tok, jnp.int32).at[dest].set(t_s)
    gate_buf = jnp.zeros((m_pad,), jnp.float32).at[dest].set(g_s)
    block_exp = jnp.minimum(
        jnp.searchsorted(pad_end, jnp.arange(n_blocks) * MOE_BLOCK, side="right"), N_EXPERTS - 1)
    h_pad = jnp.concatenate([hf, jnp.zeros((1, d), hf.dtype)], axis=0)
    xb = h_pad[tok_buf].reshape(n_blocks, MOE_BLOCK, d)

    def expert_block(args):
        x_blk, e = args
        gu = x_blk @ w1[e] + b1[e]
        gate, up = jnp.split(gu, 2, axis=-1)
        gate = jnp.minimum(gate, SWIGLU_LIMIT)
        up = jnp.clip(up, -SWIGLU_LIMIT, SWIGLU_LIMIT)
        act = gate * jax.nn.sigmoid(SWIGLU_ALPHA * gate) * (up + 1.0)
        return act @ w2[e] + b2[e]

    yb = lax.map(expert_block, (xb, block_exp)).reshape(m_pad, d)
    yb = yb * gate_buf[:, None].astype(yb.dtype)
    y = jax.ops.segment_sum(yb, tok_buf, num_segments=n_tok + 1)[:n_tok]
    return y.reshape(bsz, t, d)


def setup_inputs(seed: int = 0) -> dict:
    key = jax.random.key(seed)
    ks = jax.random.split(key, 24)
    nrm = lambda k, shape, s: jax.random.normal(k, shape, jnp.float32) * s
    dt = jnp.exp(jax.random.uniform(ks[6], (DEPTH, 2, DN_HEADS), jnp.float32,
                                    math.log(1e-3), math.log(1e-1)))
    return {
        "x": nrm(ks[0], (BATCH, SEQ, D_MODEL), 1.0),
        "c": nrm(ks[1], (BATCH, D_MODEL), 1.0),
        "ada_w": nrm(ks[2], (DEPTH, D_MODEL, 6 * D_MODEL), 0.5 * D_MODEL ** -0.5),
        "ada_b": nrm(ks[3], (DEPTH, 6 * D_MODEL), 0.02),
        "norm1_w": 1.0 + nrm(ks[4], (DEPTH, D_MODEL), 0.02),
        "w_in": nrm(ks[5], (DEPTH, D_MODEL, IN_WIDTH), D_MODEL ** -0.5),
        "conv_qkv_w": nrm(ks[7], (DEPTH, DN_CONV, 3 * DN_WIDTH), DN_CONV ** -0.5),
        "a_log": jnp.log(jax.random.uniform(ks[8], (DEPTH, 2, DN_HEADS), jnp.float32, 1.0, 16.0)),
        "dt_bias": dt + jnp.log(-jnp.expm1(-dt)),
        "onorm_w": 1.0 + nrm(ks[9], (DEPTH, DN_HEAD_DIM), 0.02),
        "w_up_a": nrm(ks[10], (DEPTH, DN_WIDTH, D_MODEL), DN_WIDTH ** -0.5),
        "conv_sc_w": nrm(ks[11], (DEPTH, SC_CONV, SC_WIDTH), SC_CONV ** -0.5),
        "w_out_sc": nrm(ks[12], (DEPTH, SC_WIDTH, D_MODEL), SC_WIDTH ** -0.5),
        "w_o": nrm(ks[13], (DEPTH, D_MODEL, D_MODEL), D_MODEL ** -0.5),
        "norm2_w": 1.0 + nrm(ks[14], (DEPTH, D_MODEL), 0.02),
        "router_w": nrm(ks[15], (DEPTH, D_MODEL, N_EXPERTS), D_MODEL ** -0.5),
        "router_b": nrm(ks[16], (DEPTH, N_EXPERTS), 0.01),
        "moe_w1": nrm(ks[17], (DEPTH, N_EXPERTS, D_MODEL, 2 * D_FF), D_MODEL ** -0.5),
        "moe_b1": nrm(ks[18], (DEPTH, N_EXPERTS, 2 * D_FF), 0.01),
        "moe_w2": nrm(ks[19], (DEPTH, N_EXPERTS, D_FF, D_MODEL), D_FF ** -0.5),
        "moe_b2": nrm(ks[20], (DEPTH, N_EXPERTS, D_MODEL), 0.01),
        "final_norm_w": 1.0 + nrm(ks[21], (D_MODEL,), 0.02),
    }


def reference(x, c, ada_w, ada_b, norm1_w, w_in, conv_qkv_w, a_log, dt_bias, onorm_w, w_up_a,
              conv_sc_w, w_out_sc, w_o, norm2_w, router_w, router_b, moe_w1, moe_b1, moe_w2,
              moe_b2, final_norm_w):
    c_act = jax.nn.silu(c)
    for l in range(DEPTH):
        mod = c_act @ ada_w[l] + ada_b[l]
        sh_m, sc_m, gt_m, sh_f, sc_f, gt_f = (m[:, None, :] for m in jnp.split(mod, 6, axis=-1))
        h = rmsnorm(x, norm1_w[l]) * (1.0 + sc_m) + sh_m
        x = x + gt_m * hybrid_mixer(h, w_in[l], conv_qkv_w[l], a_log[l], dt_bias[l], onorm_w[l],
                                    w_up_a[l], conv_sc_w[l], w_out_sc[l], w_o[l])
        h = rmsnorm(x, norm2_w[l]) * (1.0 + sc_f) + sh_f
        x = x + gt_f * moe_ffn(h, router_w[l], router_b[l], moe_w1[l], moe_b1[l], moe_w2[l], moe_b2[l])
    return rmsnorm(x, final_norm_w)
```

```python
import numpy as np
from contextlib import ExitStack
import concourse.bass as bass
import concourse.mybir as mybir
from concourse.bass_utils import run_bass_kernel_spmd

F32 = mybir.dt.float32
BF16 = mybir.dt.bfloat16
I32 = mybir.dt.int32
U32 = mybir.dt.uint32
AF = mybir.ActivationFunctionType
ALU = mybir.AluOpType
AX = mybir.AxisListType

P = 128
D = 1024
KC = 8
NH = 8
NE = 32
EPS = 1e-6
CAPF = 3.0


class Sem:
    def __init__(self, h):
        self.h = h
        self.n = 0
        self.is_dma = False


class Tl:
    def __init__(self, t):
        self.t = t
        self.w = {}
        self.r = {}

    def __getitem__(self, k):
        return self.t[k]


class Eng:
    def __init__(self, h, sem, same):
        self.h = h
        self.sem = sem
        self.seen = {}
        self.same = same


class K:
    def __init__(self, nc):
        self.nc = nc
        self.es = ExitStack()
        self.nsem = 0
        mk = lambda h, nm, same: Eng(h, self.newsem(nm), same)
        self.pe = mk(nc.tensor, "pe", False)
        self.act = mk(nc.scalar, "act", True)
        self.dve = mk(nc.vector, "dve", True)
        self.pool = mk(nc.gpsimd, "pool", True)
        self.sp = mk(nc.sync, "sp", False)
        self.engs = [self.pe, self.act, self.dve, self.pool, self.sp]
        self.esems = [e.sem for e in self.engs[:4]]
        self.dsems = []

    def newsem(self, name):
        self.nsem += 1
        return Sem(self.es.enter_context(self.nc.semaphore(f"{name}_{self.nsem}")))

    def dsem(self, name):
        s = self.newsem("d" + name)
        s.is_dma = True
        self.dsems.append(s)
        return s

    def sb(self, name, shape, dt, es=None):
        return Tl((es or self.es).enter_context(self.nc.sbuf_tensor(name, list(shape), dt)))

    def ps(self, name, shape, dt, es=None):
        return Tl((es or self.es).enter_context(self.nc.psum_tensor(name, list(shape), dt)))

    def _deps(self, eng, R, W):
        deps = {}

        def need(d):
            for s, v in d.items():
                if v > deps.get(s, 0):
                    deps[s] = v
        for t in R:
            need(t.w)
        for t in W:
            need(t.w)
            need(t.r)
        for s, v in deps.items():
            if s.is_dma:
                v = s.n
            if s is eng.sem and not eng.same:
                continue
            if eng.seen.get(s, 0) >= v:
                continue
            eng.h.wait_ge(s.h, v)
            eng.seen[s] = v

    def _stamp(self, s, R, W):
        for t in R:
            t.r[s] = s.n
        for t in W:
            t.w[s] = s.n
            t.r = {}

    SEM_LIMIT = 12000

    def op(self, eng, fn, R=(), W=()):
        if eng.sem.n >= self.SEM_LIMIT:
            eng.sem = self.newsem("roll")
            self.esems.append(eng.sem)
        self._deps(eng, R, W)
        inst = fn(eng.h)
        eng.sem.n += 1
        inst.then_inc(eng.sem.h, 1)
        self._stamp(eng.sem, R, W)

    def dma(self, eng, fn, ds=None, R=(), W=()):
        if ds is None:
            ds = self.dsem("os")
        self._deps(eng, R, W)
        inst = fn(eng.h)
        ds.n += 16
        inst.then_inc(ds.h, 16)
        self._stamp(ds, R, W)

    def barrier(self):
        sems = self.esems + self.dsems
        for e in self.engs:
            for s in sems:
                if s.n > 0 and e.seen.get(s, 0) < s.n and not (s is e.sem and not e.same):
                    e.h.wait_ge(s.h, s.n)
                    e.seen[s] = s.n


def CP(eng, out, in_):
    if hasattr(eng.h, "tensor_copy"):
        return lambda h: h.tensor_copy(out=out, in_=in_)
    return lambda h: h.activation(out=out, in_=in_, func=AF.Copy)


def _consts():
    i = np.arange(P)
    same = (i[:, None] // 64) == (i[None, :] // 64)
    c = {}
    c["ident"] = np.eye(P, dtype=np.float32)
    c["U"] = (same & (i[:, None] <= i[None, :])).astype(np.float32)
    c["SUx"] = (same & (i[:, None] > i[None, :])).astype(np.float32)
    c["OC0"] = np.repeat((i < 64).astype(np.float32)[:, None], P, 1)
    c["OC1"] = np.repeat((i >= 64).astype(np.float32)[:, None], P, 1)
    c["SLneg"] = -(same & (i[:, None] > i[None, :])).astype(np.float32)
    c["SUneg"] = -(same & (i[:, None] < i[None, :])).astype(np.float32)
    c["SUinc"] = (same & (i[:, None] <= i[None, :])).astype(np.float32)
    c["SUfull"] = (i[:, None] < i[None, :]).astype(np.float32)
    c["ONES"] = np.ones((P, P), np.float32)
    names = ["ident", "U", "SUx", "OC0", "OC1", "SLneg", "SUneg", "SUinc", "SUfull", "ONES"]
    return names, np.stack([c[n] for n in names], axis=1)


def build(TSEQ, debug=False, stop=99):
    NT = TSEQ // P
    NG = NT // 4
    TQ = TSEQ // 4
    NTQ = TQ // P
    nc = bass.Bass("TRN2", target_bir_lowering=False)
    k = K(nc)
    pe, act, dve, pool, sp = k.pe, k.act, k.dve, k.pool, k.sp
    din = lambda n, s, d=F32: nc.dram_tensor(n, list(s), d, kind="ExternalInput").ap()
    xs = [din("xf", [TSEQ, D]), din("xr", [TSEQ, D])]
    c_fm = din("c_fm", [P, KC])
    ada_w = din("ada_w", [D, 6 * D])
    ada_b_fm = din("ada_b_fm", [P, 48])
    n1_fm = din("n1_fm", [P, KC])
    w_qkv = din("w_qkv", [D, 3 * D])
    w_bd = din("w_bd", [D, 32])
    convq_fm = din("convq_fm", [P, 24, 5])
    negA_dt = din("negA_dt", [1, 64])
    consts = din("consts", [P, 10, P])
    o_kind = "ExternalOutput" if debug else "Internal"
    o_dram = [Tl(nc.dram_tensor(f"o_d{d}", [TSEQ, D], F32, kind=o_kind).ap()) for d in range(2)]

    dbg = {}
    if debug:
        dbg["gt"] = Tl(nc.dram_tensor("dbg_gt", [P, 40], F32, kind="ExternalOutput").ap())
        dbg["eg"] = Tl(nc.dram_tensor("dbg_eg", [P, 32], F32, kind="ExternalOutput").ap())
        dbg["mod"] = Tl(nc.dram_tensor("dbg_mod", [P, 48], F32, kind="ExternalOutput").ap())
        dbg["biasq"] = Tl(nc.dram_tensor("dbg_biasq", [P, 24], F32, kind="ExternalOutput").ap())
        dbg["hT"] = Tl(nc.dram_tensor("dbg_hT", [P, KC, 516], BF16, kind="ExternalOutput").ap())
        dbg["ssx"] = Tl(nc.dram_tensor("dbg_ssx", [P, 1], F32, kind="ExternalOutput").ap())
        dbg["hb"] = Tl(nc.dram_tensor("dbg_hb", [P, D], BF16, kind="ExternalOutput").ap())
        dbg["xt"] = Tl(nc.dram_tensor("dbg_xt", [P, D], F32, kind="ExternalOutput").ap())
        dbg["sqs"] = Tl(nc.dram_tensor("dbg_sqs", [P, D], F32, kind="ExternalOutput").ap())
        dbg["qkv"] = Tl(nc.dram_tensor("dbg_qkv", [P, 24, 512], BF16, kind="ExternalOutput").ap())
    es0 = ExitStack()
    cst = k.sb("cst", [P, 10, P], F32)
    cstb = k.sb("cstb", [P, 10, P], BF16)
    d_c = k.dsem("c")
    k.dma(sp, lambda h: h.dma_start(out=cst[:], in_=consts), None, W=[cst])
    k.op(dve, lambda h: h.tensor_copy(out=cstb[:], in_=cst[:]), R=[cst], W=[cstb])
    ident_f, U_f, SUx_f, OC0_f, OC1_f, SLneg_f, SUneg_f, SUinc_f = [cst[:, i, :] for i in range(8)]
    ident_b = cstb[:, 0, :]

    mod = k.sb("mod", [P, 48], F32)
    s1 = k.sb("s1", [P, KC], F32)
    cact = k.sb("cact", [P, KC], F32)
    with ExitStack() as es:
        cf = k.sb("cf", [P, KC], F32, es)
        adab = k.sb("adab", [P, 48], F32, es)
        n1 = k.sb("n1", [P, KC], F32, es)
        aw = [k.sb(f"aw{i}", [P, KC, 768], F32, es) for i in range(2)]
        d_aw = [k.dsem(f"aw{i}") for i in range(2)]
        pmod = k.ps("pmod", [P, 512], F32, es)
        k.dma(sp, lambda h: h.dma_start(out=cf[:], in_=c_fm), None, W=[cf])
        k.dma(sp, lambda h: h.dma_start(out=adab[:], in_=ada_b_fm), None, W=[adab])
        k.dma(sp, lambda h: h.dma_start(out=n1[:], in_=n1_fm), None, W=[n1])
        k.op(act, lambda h: h.activation(out=cact[:], in_=cf[:], func=AF.Silu), R=[cf], W=[cact])
        for blk in range(8):
            a = aw[blk % 2]
            k.dma(sp, lambda h: h.dma_start(out=a[:], in_=ada_w[:, blk * 768:(blk + 1) * 768].rearrange("(k p) c -> p k c", p=P)), d_aw[blk % 2], W=[a])
            for j in range(6):
                nk = blk * 6 + j
                for kc in range(KC):
                    k.op(pe, lambda h: h.matmul(pmod[:, nk:nk + 1], lhsT=a[:, kc, j * P:(j + 1) * P], rhs=cact[:, kc:kc + 1],
                                                start=(kc == 0), stop=(kc == KC - 1)), R=[a, cact], W=[pmod])
        k.op(dve, lambda h: h.tensor_tensor(out=mod[:], in0=pmod[:, 0:48], in1=adab[:], op=ALU.add), R=[pmod, adab], W=[mod])
        k.op(dve, lambda h: h.scalar_tensor_tensor(out=s1[:], in0=mod[:, 8:16], scalar=1.0, in1=n1[:], op0=ALU.add, op1=ALU.mult),
             R=[mod, n1], W=[s1])
        k.barrier()
    sh_m = lambda kc: mod[:, kc:kc + 1]

    es1 = ExitStack()
    wq = k.sb("wq", [P, KC, 3 * D], BF16, es1)
    wbd = k.sb("wbd", [P, KC, 32], BF16, es1)
    bias_q = k.sb("bias_q", [P, 24], F32, es1)
    bias_bd = k.sb("bias_bd", [P, 32], F32, es1)
    cq = k.sb("cq", [P, 24, 5], F32, es1)
    bsum = k.sb("bsum", [P, 24], F32, es1)
    gpar = k.sb("gpar", [P, 64], F32, es1)
    with ExitStack() as es:
        wst = [k.sb(f"wst{i}", [P, 3 * D], F32, es) for i in range(2)]
        d_w = [k.dsem(f"w{i}") for i in range(2)]
        wbs = k.sb("wbs", [P, KC, 32], F32, es)
        shb = k.sb("shb", [P, KC, P], F32, es)
        pb = k.ps("pb", [P, 512], F32, es)
        pb2 = k.ps("pb2", [P, 512], F32, es)
        k.dma(sp, lambda h: h.dma_start(out=cq[:], in_=convq_fm), None, W=[cq])
        k.dma(sp, lambda h: h.dma_start(out=gpar[:], in_=negA_dt.partition_broadcast(P)), None, W=[gpar])
        k.dma(sp, lambda h: h.dma_start(out=wbs[:], in_=w_bd.rearrange("(k p) c -> p k c", p=P)), None, W=[wbs])
        k.op(act, lambda h: h.activation(out=gpar[:, 0:16], in_=gpar[:, 0:16], func=AF.Exp), R=[gpar], W=[gpar])
        k.op(dve, lambda h: h.tensor_scalar(out=gpar[:, 0:16], in0=gpar[:, 0:16], scalar1=-1.0, scalar2=None, op0=ALU.mult),
             R=[gpar], W=[gpar])
        for kc in range(KC):
            k.op(pool, lambda h: h.tensor_copy(out=shb[:, kc, :], in_=mod[:, kc:kc + 1].to_broadcast([P, P])), R=[mod], W=[shb])
        for kc in range(KC):
            w = wst[kc % 2]
            k.dma(sp, lambda h: h.dma_start(out=w[:], in_=w_qkv[kc * P:(kc + 1) * P, :]), d_w[kc % 2], W=[w])
            k.op(dve if kc % 2 else pool, lambda h: h.tensor_scalar(out=wq[:, kc, :], in0=w[:], scalar1=s1[:, kc:kc + 1], scalar2=None,
                                                                    op0=ALU.mult), R=[w, s1], W=[wq])
            k.op(pe, lambda h: h.matmul(pb2[:, 0:32], lhsT=shb[:, kc, :], rhs=wbs[:, kc, :], start=(kc == 0), stop=(kc == KC - 1)),
                 R=[shb, wbs], W=[pb2])
            k.op(dve, lambda h: h.tensor_scalar(out=wbd[:, kc, :], in0=wbs[:, kc, :], scalar1=s1[:, kc:kc + 1], scalar2=None, op0=ALU.mult),
                 R=[wbs, s1], W=[wbd])
        for blk in range(8):
            w = wst[blk % 2]
            wv = w[:].rearrange("p (k c) -> p k c", k=KC)
            k.dma(sp, lambda h: h.dma_start(out=wv, in_=w_qkv[:, blk * 384:(blk + 1) * 384].rearrange("(k p) c -> p k c", p=P)), d_w[blk % 2], W=[w])
            for j in range(3):
                cc = blk * 3 + j
                for kc in range(KC):
                    k.op(pe, lambda h: h.matmul(pb[:, cc:cc + 1], lhsT=wv[:, kc, j * P:(j + 1) * P], rhs=sh_m(kc),
                                                start=(kc == 0), stop=(kc == KC - 1)), R=[w, mod], W=[pb])
        k.op(dve, lambda h: h.tensor_copy(out=bias_q[:], in_=pb[:, 0:24]), R=[pb], W=[bias_q])
        k.op(dve, lambda h: h.tensor_copy(out=bias_bd[:], in_=pb2[:, 0:32]), R=[pb2], W=[bias_bd])
        k.op(dve, lambda h: h.tensor_reduce(out=bsum[:], in_=cq[:], axis=AX.X, op=ALU.add), R=[cq], W=[bsum])
        k.op(dve, lambda h: h.tensor_tensor(out=bsum[:], in0=bsum[:], in1=bias_q[:], op=ALU.mult), R=[bsum, bias_q], W=[bsum])
        k.barrier()

    with ExitStack() as es:
        psf = [k.ps(f"psf{i}", [P, 4, P], F32, es) for i in range(6)]
        psb_t = [k.ps(f"psb{i}", [P, 2, 4, P], BF16, es) for i in range(2)]
        psb = []
        for t in psb_t:
            psb.append((t, 0))
            psb.append((t, 1))
        rr = {"f": 0, "b": 0}

        def nf():
            rr["f"] += 1
            return psf[rr["f"] % 6]

        def nb():
            rr["b"] += 1
            t, s = psb[rr["b"] % 4]
            return t, s

        xt = [k.sb(f"xt{i}", [P, D], F32, es) for i in range(2)]
        d_x = [k.dsem(f"x{i}") for i in range(2)]
        ssx = [k.sb(f"ssx{i}", [P, 1], F32, es) for i in range(2)]
        sqs = k.sb("sqs", [P, D], F32, es)
        hb = [k.sb(f"hb{i}", [P, D], BF16, es) for i in range(2)]
        hT = [k.sb(f"hT{i}", [P, KC, 516], BF16, es) for i in range(2)]
        hlast = [k.sb(f"hlast{i}", [P, KC, 2], BF16, es) for i in range(3)]
        praw = [k.sb(f"praw{i}", [P, 516], F32, es) for i in range(2)]
        cva = [k.sb(f"cva{i}", [P, 512], F32, es) for i in range(1)]
        qkv = [k.sb(f"qkv{i}", [P, 24, 512], BF16, es) for i in range(2)]
        bdt = [k.sb(f"bdt{i}", [P, 16], F32, es) for i in range(8)]
        gt = [k.sb(f"gt{i}", [P, 40], F32, es) for i in range(8)]
        eG = [k.sb(f"eG{i}", [P, 32], F32, es) for i in range(8)]
        S = [k.sb(f"S{i}", [P, 4, P], F32, es) for i in range(2)]
        Sb = [k.sb(f"Sb{i}", [P, 4, P], BF16, es) for i in range(2)]
        o_sb = [k.sb(f"o_sb{i}", [P, NH, P], F32, es) for i in range(2)]
        d_o = [k.dsem(f"o{i}") for i in range(2)]

        SINGLE = {"sq", "Gm"}

        NSET = 2

        def dbl(name, shape, dt):
            if name in SINGLE:
                t = k.sb(f"{name}0", shape, dt, es)
                return [t] * NSET
            return [k.sb(f"{name}{i}", shape, dt, es) for i in range(NSET)]
        T3 = [P, 4, P]
        k_tm, v_tm, q_tm = dbl("k_tm", T3, BF16), dbl("v_tm", T3, BF16), dbl("q_tm", T3, BF16)
        sq = dbl("sq", T3, F32)
        ss = dbl("ss", [P, 8], F32)
        sc = dbl("sc", [P, 24], F32)
        khat, kb_, kbg, kdec, qhat, qd, vb = [dbl(n, T3, BF16) for n in ("khat", "kb_", "kbg", "kdec", "qhat", "qd", "vb")]
        khatT, kbT, qhatT, qdT = [dbl(n, T3, BF16) for n in ("khatT", "kbT", "qhatT", "qdT")]
        Gm = sq
        ED, EDT = dbl("ED", T3, F32), dbl("EDT", T3, F32)
        EDTs = dbl("EDTs", T3, F32)
        EDm, EDTi = ED, EDT
        nA, nTA = qhat, qd
        nB, nTB = k_tm, q_tm
        AT = dbl("AT", T3, BF16)
        PTa, PTb = dbl("PTa", T3, BF16), v_tm
        u_sb = ED
        wT = khat
        vnb = kb_

        def rsqrt_(tl, ap, eps, mul=1.0):
            k.op(dve, lambda h: h.tensor_scalar(out=ap, in0=ap, scalar1=float(mul), scalar2=float(eps), op0=ALU.mult, op1=ALU.add), R=[tl], W=[tl])
            k.op(act, lambda h: h.activation(out=ap, in_=ap, func=AF.Ln), R=[tl], W=[tl])
            k.op(act, lambda h: h.activation(out=ap, in_=ap, func=AF.Exp, scale=-0.5), R=[tl], W=[tl])

        def bc_h(ap):
            return ap.unsqueeze(2).to_broadcast([P, 4, P])

        def bc_m(ap):
            return ap.unsqueeze(1).to_broadcast([P, 4, P])

        uid = [0]

        for dr in range(2 if stop >= 2 else 1):
            xsrc = xs[dr]
            for i in range(2):
                k.op(pool, lambda h: h.memset(S[i][:], 0.0), W=[S[i]])
                k.op(pool, lambda h: h.memset(Sb[i][:], 0.0), W=[Sb[i]])

            def stage_x(g):
                H = hT[g % 2]
                for ti in range(4):
                    t = g * 4 + ti
                    b = t % 2
                    k.dma(sp, lambda h: h.dma_start(out=xt[b][:], in_=xsrc[t * P:(t + 1) * P, :]), d_x[b], W=[xt[b]])
                    k.op(act, lambda h: h.activation(out=sqs[:], in_=xt[b][:], func=AF.Square), R=[xt[b]], W=[sqs])
                    k.op(dve, lambda h: h.tensor_reduce(out=ssx[b][:], in_=sqs[:], axis=AX.X, op=ALU.add), R=[sqs], W=[ssx[b]])
                    rsqrt_(ssx[b], ssx[b][:], EPS, 1.0 / D)
                    k.op(act, lambda h: h.activation(out=hb[b][:], in_=xt[b][:], func=AF.Copy, scale=ssx[b][:, 0:1]), R=[xt[b], ssx[b]], W=[hb[b]])
                    if debug and dr == 0 and t == 0:
                        k.dma(sp, lambda h: h.dma_start(out=dbg["ssx"][:], in_=ssx[b][:]), None, R=[ssx[b]], W=[dbg["ssx"]])
                        k.dma(sp, lambda h: h.dma_start(out=dbg["hb"][:], in_=hb[b][:]), None, R=[hb[b]], W=[dbg["hb"]])
                        k.dma(sp, lambda h: h.dma_start(out=dbg["xt"][:], in_=xt[b][:]), None, R=[xt[b]], W=[dbg["xt"]])
                        k.dma(sp, lambda h: h.dma_start(out=dbg["sqs"][:], in_=sqs[:]), None, R=[sqs], W=[dbg["sqs"]])
                    for half in range(2):
                        pt, s = nb()
                        for j in range(4):
                            kc = half * 4 + j
                            k.op(pe, lambda h: h.transpose(out=pt[:, s, j, :], in_=hb[b][:, kc * P:(kc + 1) * P], identity=ident_b),
                                 R=[hb[b], cstb], W=[pt])
                        k.op(act if half else dve, CP(act if half else dve, H[:, half * 4:half * 4 + 4, 2 + ti * P:2 + (ti + 1) * P], pt[:, s, :, :]), R=[pt], W=[H])
                k.op(pool, lambda h: h.tensor_copy(out=hlast[g % 3][:], in_=H[:, :, 512:514]), R=[H], W=[hlast[g % 3]])

            def stage_proj(g):
                H = hT[g % 2]
                Q = qkv[g % 2]
                if g > 0:
                    Hp = hlast[(g - 1) % 3]
                    k.op(pool, lambda h: h.tensor_copy(out=H[:, :, 0:2], in_=Hp[:]), R=[Hp], W=[H])
                else:
                    k.op(pool, lambda h: h.memset(H[:, :, 0:2], 0.0), W=[H])
                if g < NG - 1:
                    Hn = hT[(g + 1) % 2]
                    k.op(pool, lambda h: h.tensor_copy(out=H[:, :, 514:516], in_=Hn[:, :, 2:4]), R=[Hn], W=[H])
                else:
                    k.op(pool, lambda h: h.memset(H[:, :, 514:516], 0.0), W=[H])
                for cc in range(24):
                    pm = nf()
                    pmf = pm[:].rearrange("p a b -> p (a b)")
                    ph = nf()
                    phf = ph[:].rearrange("p a b -> p (a b)")
                    for kc in range(KC):
                        k.op(pe, lambda h: h.matmul(pmf, lhsT=wq[:, kc, cc * P:(cc + 1) * P], rhs=H[:, kc, 0:512],
                                                    start=(kc == 0), stop=(kc == KC - 1)), R=[wq, H], W=[pm])
                    for kc in range(KC):
                        k.op(pe, lambda h: h.matmul(phf[:, 0:4], lhsT=wq[:, kc, cc * P:(cc + 1) * P], rhs=H[:, kc, 512:516],
                                                    start=(kc == 0), stop=(kc == KC - 1)), R=[wq, H], W=[ph])
                    pr = praw[cc % 2]
                    k.op(act, lambda h: h.activation(out=pr[:, 0:512], in_=pmf, func=AF.Copy), R=[pm], W=[pr])
                    k.op(act, lambda h: h.activation(out=pr[:, 512:516], in_=phf[:, 0:4], func=AF.Copy), R=[ph], W=[pr])
                    ca = cva[0]
                    tp = (lambda t_: cq[:, cc, t_:t_ + 1]) if dr == 0 else (lambda t_: cq[:, cc, 4 - t_:5 - t_])
                    k.op(dve, lambda h: h.tensor_scalar(out=ca[:], in0=pr[:, 0:512], scalar1=tp(0), scalar2=None, op0=ALU.mult), R=[pr, cq], W=[ca])
                    k.op(dve, lambda h: h.scalar_tensor_tensor(out=ca[:], in0=pr[:, 1:513], scalar=tp(1), in1=ca[:], op0=ALU.mult, op1=ALU.add),
                         R=[pr, cq, ca], W=[ca])
                    k.op(dve, lambda h: h.scalar_tensor_tensor(out=ca[:], in0=pr[:, 2:514], scalar=tp(2), in1=ca[:], op0=ALU.mult, op1=ALU.add),
                         R=[pr, cq, ca], W=[ca])
                    k.op(dve, lambda h: h.scalar_tensor_tensor(out=ca[:], in0=pr[:, 3:515], scalar=tp(3), in1=ca[:], op0=ALU.mult, op1=ALU.add),
                         R=[pr, cq, ca], W=[ca])
                    k.op(dve, lambda h: h.scalar_tensor_tensor(out=ca[:], in0=pr[:, 4:516], scalar=tp(4), in1=ca[:], op0=ALU.mult, op1=ALU.add),
                         R=[pr, cq, ca], W=[ca])
                    if g == 0:
                        for t_ in range(2):
                            for m in range(2 - t_):
                                k.op(dve, lambda h: h.scalar_tensor_tensor(out=ca[:, t_:t_ + 1], in0=bias_q[:, cc:cc + 1], scalar=tp(m), in1=ca[:, t_:t_ + 1],
                                                                           op0=ALU.mult, op1=ALU.subtract), R=[bias_q, cq, ca], W=[ca])
                                k.op(dve, lambda h: h.tensor_scalar(out=ca[:, t_:t_ + 1], in0=ca[:, t_:t_ + 1], scalar1=-1.0, scalar2=None, op0=ALU.mult),
                                     R=[ca], W=[ca])
                    if g == NG - 1:
                        for t_ in range(2):
                            col = 511 - t_
                            for m in range(2 - t_):
                                k.op(dve, lambda h: h.scalar_tensor_tensor(out=ca[:, col:col + 1], in0=bias_q[:, cc:cc + 1], scalar=tp(4 - m),
                                                                           in1=ca[:, col:col + 1], op0=ALU.mult, op1=ALU.subtract), R=[bias_q, cq, ca], W=[ca])
                                k.op(dve, lambda h: h.tensor_scalar(out=ca[:, col:col + 1], in0=ca[:, col:col + 1], scalar1=-1.0, scalar2=None, op0=ALU.mult),
                                     R=[ca], W=[ca])
                    k.op(act, lambda h: h.activation(out=Q[:, cc, :], in_=ca[:], func=AF.Silu, bias=bsum[:, cc:cc + 1]), R=[ca, bsum], W=[Q])
                    yield

            def stage_gates(g, ti):
                H = hT[g % 2]
                t = g * 4 + ti
                b = (g % 2) * 4 + ti
                pm = nf()
                pmf = pm[:].rearrange("p a b -> p (a b)")
                for kc in range(KC):
                    k.op(pe, lambda h: h.matmul(pmf[:, 0:16], lhsT=H[:, kc, 2 + ti * P:2 + (ti + 1) * P], rhs=wbd[:, kc, dr * 16:(dr + 1) * 16],
                                                start=(kc == 0), stop=(kc == KC - 1)), R=[H, wbd], W=[pm])
                B, Gt, Eg = bdt[b], gt[b], eG[b]
                k.op(dve, lambda h: h.tensor_tensor(out=B[:], in0=pmf[:, 0:16], in1=bias_bd[:, dr * 16:(dr + 1) * 16], op=ALU.add),
                     R=[pm, bias_bd], W=[B])
                k.op(act, lambda h: h.activation(out=Gt[:, 0:8], in_=B[:, 0:8], func=AF.Exp, scale=-1.0), R=[B], W=[Gt])
                k.op(dve, lambda h: h.tensor_scalar(out=Gt[:, 0:8], in0=Gt[:, 0:8], scalar1=1.0, scalar2=None, op0=ALU.add), R=[Gt], W=[Gt])
                k.op(dve, lambda h: h.reciprocal(out=Gt[:, 8:16], in_=Gt[:, 0:8]), R=[Gt], W=[Gt])
                k.op(dve, lambda h: h.tensor_tensor(out=Gt[:, 16:24], in0=B[:, 8:16], in1=gpar[:, 16 + dr * 8:24 + dr * 8], op=ALU.add),
                     R=[B, gpar], W=[Gt])
                k.op(act, lambda h: h.activation(out=Gt[:, 16:24], in_=Gt[:, 16:24], func=AF.Exp), R=[Gt], W=[Gt])
                k.op(act, lambda h: h.activation(out=Gt[:, 16:24], in_=Gt[:, 16:24], func=AF.Ln, bias=1.0), R=[Gt], W=[Gt])
                k.op(dve, lambda h: h.tensor_tensor(out=Gt[:, 24:32], in0=Gt[:, 16:24], in1=gpar[:, dr * 8:dr * 8 + 8], op=ALU.mult),
                     R=[Gt, gpar], W=[Gt])
                pg = nf()
                pgf = pg[:].rearrange("p a b -> p (a b)")
                for i, m in enumerate((U_f, SUx_f, OC0_f, OC1_f)):
                    k.op(pe, lambda h: h.matmul(pgf[:, i * 8:(i + 1) * 8], lhsT=m, rhs=Gt[:, 24:32], start=True, stop=True), R=[cst, Gt], W=[pg])
                k.op(act, lambda h: h.activation(out=Eg[:], in_=pgf[:, 0:32], func=AF.Exp), R=[pg], W=[Eg])
                return Gt, Eg

            def unit(g, ti, hg, Gt, Eg):
                uid[0] += 1
                z = uid[0] % NSET
                Q = qkv[g % 2]
                cs = slice(ti * P, (ti + 1) * P)
                hs = slice(4 * hg, 4 * hg + 4)
                beta = Gt[:, 8 + 4 * hg:12 + 4 * hg]
                gg = Gt[:, 24 + 4 * hg:28 + 4 * hg]
                eGc = Eg[:, 4 * hg:4 * hg + 4]
                eR = Eg[:, 8 + 4 * hg:12 + 4 * hg]
                for base, dst, eng in ((8, k_tm[z], act), (16, v_tm[z], act), (0, q_tm[z], act)):
                    pt, s = nb()
                    for j in range(4):
                        k.op(pe, lambda h: h.transpose(out=pt[:, s, j, :], in_=Q[:, base + 4 * hg + j, cs], identity=ident_b), R=[Q, cstb], W=[pt])
                    k.op(eng, CP(eng, dst[:], pt[:, s, :, :]), R=[pt], W=[dst])
                    yield
                SS, SC = ss[z], sc[z]
                k.op(act, lambda h: h.activation(out=sq[z][:], in_=k_tm[z][:], func=AF.Square), R=[k_tm[z]], W=[sq[z]])
                k.op(dve, lambda h: h.tensor_reduce(out=SS[:, 0:4], in_=sq[z][:], axis=AX.X, op=ALU.add), R=[sq[z]], W=[SS])
                k.op(act, lambda h: h.activation(out=sq[z][:], in_=q_tm[z][:], func=AF.Square), R=[q_tm[z]], W=[sq[z]])
                k.op(dve, lambda h: h.tensor_reduce(out=SS[:, 4:8], in_=sq[z][:], axis=AX.X, op=ALU.add), R=[sq[z]], W=[SS])
                rsqrt_(SS, SS[:], EPS)
                yield
                rk, rq = SS[:, 0:4], SS[:, 4:8]
                tt = lambda o, a, b_: k.op(dve, lambda h: h.tensor_tensor(out=o, in0=a, in1=b_, op=ALU.mult), R=[SS, SC, Gt, Eg], W=[SC])
                s_kb, s_kbg, s_kd, s_q, s_qd = [SC[:, 4 * i:4 * i + 4] for i in range(5)]
                tt(s_kb, rk, beta)
                tt(s_kbg, s_kb, eGc)
                tt(s_kd, rk, eR)
                k.op(dve, lambda h: h.tensor_scalar(out=s_q, in0=rq, scalar1=float(P) ** -0.5, scalar2=None, op0=ALU.mult), R=[SS], W=[SC])
                tt(s_qd, s_q, eGc)
                def scl(eng, dst, src, s_ap, extra):
                    if eng is pool:
                        eng = dve
                    k.op(eng, lambda h: h.tensor_tensor(out=dst[:], in0=src[:], in1=bc_h(s_ap), op=ALU.mult), R=[src] + extra, W=[dst])
                scl(dve, khat[z], k_tm[z], rk, [SS])
                scl(pool, kb_[z], k_tm[z], s_kb, [SC])
                scl(dve, kbg[z], k_tm[z], s_kbg, [SC])
                scl(pool, kdec[z], k_tm[z], s_kd, [SC])
                scl(dve, qhat[z], q_tm[z], s_q, [SC])
                scl(pool, qd[z], q_tm[z], s_qd, [SC])
                scl(pool, vb[z], v_tm[z], beta, [Gt])
                yield
                for src, dst, eng in ((khat[z], khatT[z], act), (kb_[z], kbT[z], act), (qhat[z], qhatT[z], act), (qd[z], qdT[z], act)):
                    pt, s = nb()
                    for j in range(4):
                        k.op(pe, lambda h: h.transpose(out=pt[:, s, j, :], in_=src[:, j, :], identity=ident_b), R=[src, cstb], W=[pt])
                    k.op(eng, CP(eng, dst[:], pt[:, s, :, :]), R=[pt], W=[dst])
                    yield
                k.op(dve, lambda h: h.tensor_tensor(out=Gm[z][:], in0=bc_m(SUx_f), in1=bc_h(gg), op=ALU.mult), R=[cst, Gt], W=[Gm[z]])
                pD, pDT = nf(), nf()
                for j in range(4):
                    k.op(pe, lambda h: h.matmul(pD[:, j, :], lhsT=U_f, rhs=Gm[z][:, j, :], start=True, stop=True), R=[cst, Gm[z]], W=[pD])
                for j in range(4):
                    k.op(pe, lambda h: h.matmul(pDT[:, j, :], lhsT=Gm[z][:, j, :], rhs=U_f, start=True, stop=True), R=[cst, Gm[z]], W=[pDT])
                k.op(act, lambda h: h.activation(out=ED[z][:], in_=pD[:], func=AF.Exp), R=[pD], W=[ED[z]])
                k.op(act, lambda h: h.activation(out=EDT[z][:], in_=pDT[:], func=AF.Exp), R=[pDT], W=[EDT[z]])
                yield
                k.op(dve, lambda h: h.tensor_tensor(out=EDm[z][:], in0=ED[z][:], in1=bc_m(SLneg_f), op=ALU.mult), R=[ED[z], cst], W=[EDm[z]])
                k.op(dve, lambda h: h.tensor_tensor(out=EDTs[z][:], in0=EDT[z][:], in1=bc_m(SUneg_f), op=ALU.mult), R=[EDT[z], cst], W=[EDTs[z]])
                k.op(dve, lambda h: h.tensor_tensor(out=EDTi[z][:], in0=EDT[z][:], in1=bc_m(SUinc_f), op=ALU.mult), R=[EDT[z], cst], W=[EDTi[z]])
                yield
                pN, pNT, pA = nf(), nf(), nf()
                for j in range(4):
                    k.op(pe, lambda h: h.matmul(pN[:, j, :], lhsT=kbT[z][:, j, :], rhs=khatT[z][:, j, :], start=True, stop=True),
                         R=[kbT[z], khatT[z]], W=[pN])
                for j in range(4):
                    k.op(pe, lambda h: h.matmul(pNT[:, j, :], lhsT=khatT[z][:, j, :], rhs=kbT[z][:, j, :], start=True, stop=True),
                         R=[kbT[z], khatT[z]], W=[pNT])
                for j in range(4):
                    k.op(pe, lambda h: h.matmul(pA[:, j, :], lhsT=khatT[z][:, j, :], rhs=qhatT[z][:, j, :], start=True, stop=True),
                         R=[qhatT[z], khatT[z]], W=[pA])
                k.op(dve, lambda h: h.tensor_tensor(out=nA[z][:], in0=pN[:], in1=EDm[z][:], op=ALU.mult), R=[pN, EDm[z]], W=[nA[z]])
                k.op(dve, lambda h: h.tensor_tensor(out=nTA[z][:], in0=pNT[:], in1=EDTs[z][:], op=ALU.mult), R=[pNT, EDTs[z]], W=[nTA[z]])
                k.op(dve, lambda h: h.tensor_tensor(out=AT[z][:], in0=pA[:], in1=EDTi[z][:], op=ALU.mult), R=[pA, EDTi[z]], W=[AT[z]])
                yield
                k.op(dve, lambda h: h.tensor_tensor(out=PTa[z][:], in0=nTA[z][:], in1=bc_m(ident_f), op=ALU.add), R=[nTA[z], cst], W=[PTa[z]])
                n_c, nT_c, n_n, nT_n = nA[z], nTA[z], nB[z], nTB[z]
                PT_c, PT_n = PTa[z], PTb[z]
                for it in range(5):
                    p1 = nf()
                    for j in range(4):
                        k.op(pe, lambda h: h.matmul(p1[:, j, :], lhsT=nT_c[:, j, :], rhs=n_c[:, j, :], start=True, stop=True), R=[nT_c, n_c], W=[p1])
                    if it < 4:
                        p2 = nf()
                        for j in range(4):
                            k.op(pe, lambda h: h.matmul(p2[:, j, :], lhsT=n_c[:, j, :], rhs=nT_c[:, j, :], start=True, stop=True), R=[nT_c, n_c], W=[p2])
                    k.op(act, lambda h: h.activation(out=n_n[:], in_=p1[:], func=AF.Copy), R=[p1], W=[n_n])
                    if it < 4:
                        k.op(act, lambda h: h.activation(out=nT_n[:], in_=p2[:], func=AF.Copy), R=[p2], W=[nT_n])
                    yield
                    p3 = nf()
                    for j in range(4):
                        k.op(pe, lambda h: h.matmul(p3[:, j, :], lhsT=n_n[:, j, :], rhs=PT_c[:, j, :], start=True, stop=True), R=[n_n, PT_c], W=[p3])
                    k.op(dve, lambda h: h.tensor_tensor(out=PT_n[:], in0=p3[:], in1=PT_c[:], op=ALU.add), R=[p3, PT_c], W=[PT_n])
                    yield
                    n_c, n_n = n_n, n_c
                    nT_c, nT_n = nT_n, nT_c
                    PT_c, PT_n = PT_n, PT_c
                PT = PT_c
                pU, pW = nf(), nf()
                for j in range(4):
                    k.op(pe, lambda h: h.matmul(pU[:, j, :], lhsT=PT[:, j, :], rhs=vb[z][:, j, :], start=True, stop=True), R=[PT, vb[z]], W=[pU])
                for j in range(4):
                    k.op(pe, lambda h: h.matmul(pW[:, j, :], lhsT=kbg[z][:, j, :], rhs=PT[:, j, :], start=True, stop=True), R=[PT, kbg[z]], W=[pW])
                k.op(act, lambda h: h.activation(out=u_sb[z][:], in_=pU[:], func=AF.Copy), R=[pU], W=[u_sb[z]])
                k.op(act, lambda h: h.activation(out=wT[z][:], in_=pW[:], func=AF.Copy), R=[pW], W=[wT[z]])
                yield
                while prog[hg] != g * 4 + ti:
                    yield
                O = o_sb[(g * 4 + ti) % 2]
                St, Sbt = S[hg], Sb[hg]
                for c in range(2):
                    rs = slice(64 * c, 64 * c + 64)
                    p1 = nf()
                    for j in range(4):
                        k.op(pe, lambda h: h.matmul(p1[:, j, :], lhsT=wT[z][:, j, :], rhs=Sbt[:, j, :], start=True, stop=True), R=[wT[z], Sbt], W=[p1])
                    k.op(dve, lambda h: h.tensor_tensor(out=vnb[z][rs, :, :], in0=u_sb[z][rs, :, :], in1=p1[rs, :, :], op=ALU.subtract),
                         R=[u_sb[z], p1], W=[vnb[z]])
                    yield
                    p2, p3 = nf(), nf()
                    for j in range(4):
                        k.op(pe, lambda h: h.matmul(p2[:, j, :], lhsT=qdT[z][:, j, :], rhs=Sbt[:, j, :], start=True, stop=False), R=[qdT[z], Sbt], W=[p2])
                        k.op(pe, lambda h: h.matmul(p2[:, j, :], lhsT=AT[z][rs, j, :], rhs=vnb[z][rs, j, :], start=False, stop=True), R=[AT[z], vnb[z]], W=[p2])
                    for j in range(4):
                        k.op(pe, lambda h: h.matmul(p3[:, j, :], lhsT=kdec[z][rs, j, :], rhs=vnb[z][rs, j, :], start=True, stop=True), R=[kdec[z], vnb[z]], W=[p3])
                    k.op(act, lambda h: h.activation(out=O[rs, hs, :], in_=p2[rs, :, :], func=AF.Copy), R=[p2], W=[O])
                    egt = Eg[:, 16 + 8 * c + 4 * hg:20 + 8 * c + 4 * hg]
                    k.op(dve, lambda h: h.tensor_tensor(out=St[:], in0=St[:], in1=bc_h(egt), op=ALU.mult), R=[St, Eg], W=[St])
                    k.op(dve, lambda h: h.tensor_tensor(out=St[:], in0=St[:], in1=p3[:], op=ALU.add), R=[St, p3], W=[St])
                    k.op(act, lambda h: h.activation(out=Sbt[:], in_=St[:], func=AF.Copy), R=[St], W=[Sbt])
                    yield
                prog[hg] += 1

            prog = {0: 0, 1: 0}
            stage_x(0)
            if NG > 1:
                stage_x(1)
            for _ in stage_proj(0):
                pass
            WIN = NSET
            STAGGER = 15
            todo = [(g, ti, hg) for g in range(NG) for ti in range(4) for hg in range(2)]
            active, side, done, gates = [], [], {}, {}
            since = 10 ** 9
            while todo or active or side:
                if len(active) < WIN and todo and since >= STAGGER:
                    g, ti, hg = todo.pop(0)
                    if ti == 0 and hg == 0:
                        gates[g] = [stage_gates(g, t_) for t_ in range(4)]
                        if g + 2 < NG:
                            stage_x(g + 2)
                        if g + 1 < NG:
                            side.append(stage_proj(g + 1))
                        if debug and dr == 0 and g == 0:
                            Gt, Eg = gates[0][0]
                            k.dma(sp, lambda h: h.dma_start(out=dbg["gt"][:], in_=Gt[:]), None, R=[Gt], W=[dbg["gt"]])
                            k.dma(sp, lambda h: h.dma_start(out=dbg["eg"][:], in_=Eg[:]), None, R=[Eg], W=[dbg["eg"]])
                            k.dma(sp, lambda h: h.dma_start(out=dbg["qkv"][:], in_=qkv[0][:]), None, R=[qkv[0]], W=[dbg["qkv"]])
                    active.append((g, ti, unit(g, ti, hg, gates[g][ti][0], gates[g][ti][1])))
                    since = 0
                since += 1
                for item in list(active):
                    g_, ti_, gen = item
                    try:
                        next(gen)
                    except StopIteration:
                        active.remove(item)
                        done[(g_, ti_)] = done.get((g_, ti_), 0) + 1
                        if done[(g_, ti_)] == 2:
                            t = g_ * 4 + ti_
                            O = o_sb[t % 2]
                            k.dma(sp, lambda h: h.dma_start(out=o_dram[dr][t * P:(t + 1) * P, :], in_=O[:].rearrange("p a b -> p (a b)")),
                                  d_o[t % 2], R=[O], W=[o_dram[dr]])
                for sg_ in list(side):
                    try:
                        next(sg_)
                    except StopIteration:
                        side.remove(sg_)
        k.barrier()
    es1.close()

    if stop >= 3:
        phase2(nc, k, TSEQ, mod, s1, cst, cstb, o_dram, din, debug, stop)
    if stop < 5:
        name = "out" if stop < 3 else "out_probe"
        outp = Tl(nc.dram_tensor(name, [TSEQ // 4, D], F32, kind="ExternalOutput").ap())
        k.dma(sp, lambda h: h.dma_start(out=outp[0:P, 0:P], in_=cst[:, 0, :]), None, R=[cst], W=[outp])
    k.barrier()
    k.es.close()
    return nc


def phase2(nc, k, TSEQ, mod, s1, cst, cstb, o_dram, din, debug, stop=99):
    pe, act, dve, pool, sp = k.pe, k.act, k.dve, k.pool, k.sp
    TQ = TSEQ // 4
    NTQ = TQ // P
    GW = P
    NGQ = TQ // GW
    TPG = GW // P
    CAP = P * int(np.ceil(CAPF * TQ / 8 / P))
    CT = CAP // P
    NSLOT = NE * CAP
    BIG = float(NSLOT + 64)
    xo = din("xo", [TQ + 2, D])
    hmask = din("hmask", [P, 2])
    idx_in = din("idx", [P, 2, NTQ], I32)
    w_z, w_sc, w_g = din("w_z", [D, D]), din("w_sc", [D, 3 * D]), din("w_g", [D, 2 * D])
    convs_fm = din("convs_fm", [P, 8, 3])
    onorm = din("onorm", [1, P])
    w_up_a, w_out_sc, w_o = din("w_up_a", [D, D]), din("w_out_sc", [D, D]), din("w_o", [D, D])
    n2_fm = din("n2_fm", [P, KC])
    router_w = din("router_w", [D, NE])
    router_b = din("router_b", [1, NE])
    ecap = din("ecap", [1, NE])
    fnw = din("fnw", [1, D])
    moe_w1 = din("moe_w1", [NE, D, 2 * D])
    moe_b1_fm = din("moe_b1_fm", [P, NE, 16])
    moe_w2 = din("moe_w2", [NE, D, D])
    moe_b2 = din("moe_b2", [NE, D])
    out = Tl(nc.dram_tensor("out", [TQ, D], F32, kind="ExternalOutput").ap())
    x1_d = Tl(nc.dram_tensor("x1_d", [TQ, D], F32).ap())
    xbuf = Tl(nc.dram_tensor("xbuf", [NSLOT, D], BF16).ap())
    ybuf = Tl(nc.dram_tensor("ybuf", [NSLOT, D], F32).ap())
    ident_f, ident_b = cst[:, 0, :], cstb[:, 0, :]
    bc_o = nc.gpsimd.to_reg(TSEQ - 1)
    bc_s = nc.gpsimd.to_reg(NSLOT - 1)
    SUfull_b, ONES_b = cstb[:, 8, :], cstb[:, 9, :]

    def bc_h(ap, n, m):
        return ap.unsqueeze(2).to_broadcast([P, n, m])

    def rsqrt_(tl, ap, eps, mul=1.0):
        k.op(dve, lambda h: h.tensor_scalar(out=ap, in0=ap, scalar1=float(mul), scalar2=float(eps), op0=ALU.mult, op1=ALU.add), R=[tl], W=[tl])
        k.op(act, lambda h: h.activation(out=ap, in_=ap, func=AF.Ln), R=[tl], W=[tl])
        k.op(act, lambda h: h.activation(out=ap, in_=ap, func=AF.Exp, scale=-0.5), R=[tl], W=[tl])

    vbc = k.sb("vbc", [P, 4, D], F32)
    dki = k.sb("dki", [P, NTQ, 4], I32)
    gk = k.sb("gk", [P, NTQ, 4], F32)
    d_m = k.dsem("m")

    with ExitStack() as es:
        W2 = k.sb("W2", [P, KC, 6 * D], BF16, es)
        Wp = [k.sb(f"Wp{i}", [P, KC, D], BF16, es) for i in range(3)]
        bias2 = k.sb("bias2", [P, 48], F32, es)
        bz_bc = k.sb("bz_bc", [P, D], F32, es)
        cs3 = k.sb("cs3", [P, 8, 3], F32, es)
        on_bc = k.sb("on_bc", [P, P], F32, es)
        rb_bc = k.sb("rb_bc", [P, NE], F32, es)
        ec_bc = k.sb("ec_bc", [P, NE], F32, es)
        hm = k.sb("hm", [P, 2], F32, es)
        n2 = k.sb("n2", [P, KC], F32, es)
        s2 = k.sb("s2", [P, KC], F32, es)
        rw = k.sb("rw", [P, KC, NE], F32, es)
        idx = k.sb("idx_sb", [P, 2, NTQ], I32, es)
        selb = k.sb("selb", [P, NTQ, NE], BF16, es)
        psf = [k.ps(f"p2f{i}", [P, 512], F32, es) for i in range(6)]
        psb_t = [k.ps(f"p2b{i}", [P, 2, 4, P], BF16, es) for i in range(2)]
        rr = {"f": 0, "b": 0}

        def nf():
            rr["f"] += 1
            return psf[rr["f"] % 6]

        def nb():
            rr["b"] += 1
            return psb_t[(rr["b"] // 2) % 2], rr["b"] % 2

        for dst, src in ((cs3, convs_fm), (hm, hmask), (n2, n2_fm), (idx, idx_in)):
            k.dma(sp, lambda h: h.dma_start(out=dst[:], in_=src), None, W=[dst])
        k.dma(sp, lambda h: h.dma_start(out=on_bc[:], in_=onorm.partition_broadcast(P)), None, W=[on_bc])
        k.dma(sp, lambda h: h.dma_start(out=rb_bc[:], in_=router_b.partition_broadcast(P)), None, W=[rb_bc])
        k.dma(sp, lambda h: h.dma_start(out=ec_bc[:], in_=ecap.partition_broadcast(P)), None, W=[ec_bc])
        k.dma(sp, lambda h: h.dma_start(out=rw[:], in_=router_w.rearrange("(k p) e -> p k e", p=P)), None, W=[rw])
        for i, src in enumerate((w_up_a, w_out_sc, w_o)):
            k.dma(pool, lambda h: h.dma_start(out=Wp[i][:], in_=src.rearrange("(k p) c -> p k c", p=P)), None, W=[Wp[i]])
        k.op(dve, lambda h: h.scalar_tensor_tensor(out=s2[:], in0=mod[:, 32:40], scalar=1.0, in1=n2[:], op0=ALU.add, op1=ALU.mult), R=[mod, n2], W=[s2])
        with ExitStack() as esw:
            BW = 256
            wst = [k.sb(f"w2st{i}", [P, KC, BW], F32, esw) for i in range(2)]
            d_w = [k.dsem(f"w2{i}") for i in range(2)]
            shb = k.sb("shb2", [P, KC, P], F32, esw)
            vb_l = k.sb("vb_l", [P, P], F32, esw)
            pb = psf[5]
            pz = [psf[3], psf[4]]
            for kc in range(KC):
                k.op(pool, lambda h: h.tensor_copy(out=shb[:, kc, :], in_=mod[:, kc:kc + 1].to_broadcast([P, P])), R=[mod], W=[shb])
            srcs = [(w_z, c0) for c0 in range(0, D, BW)] + [(w_sc, c0) for c0 in range(0, 3 * D, BW)] + [(w_g, c0) for c0 in range(0, 2 * D, BW)]
            for blk, (src, c0) in enumerate(srcs):
                w = wst[blk % 2]
                k.dma(sp, lambda h: h.dma_start(out=w[:], in_=src[:, c0:c0 + BW].rearrange("(k p) c -> p k c", p=P)), d_w[blk % 2], W=[w])
                for j in range(BW // P):
                    cc = blk * (BW // P) + j
                    for kc in range(KC):
                        k.op(pe, lambda h: h.matmul(pb[:, cc:cc + 1], lhsT=w[:, kc, j * P:(j + 1) * P], rhs=mod[:, kc:kc + 1],
                                                    start=(kc == 0), stop=(kc == KC - 1)), R=[w, mod], W=[pb])
                if blk < D // BW:
                    pzt = pz[blk % 2]
                    for kc in range(KC):
                        k.op(pe, lambda h: h.matmul(pzt[:, 0:BW], lhsT=shb[:, kc, :], rhs=w[:, kc, :], start=(kc == 0), stop=(kc == KC - 1)),
                             R=[shb, w], W=[pzt])
                    k.op(act, CP(act, bz_bc[:, blk * BW:(blk + 1) * BW], pzt[:, 0:BW]), R=[pzt], W=[bz_bc])
                k.op(dve if blk % 2 else pool, lambda h: h.tensor_tensor(out=W2[:, :, blk * BW:(blk + 1) * BW], in0=w[:], in1=bc_h(s1[:, :], KC, BW), op=ALU.mult),
                     R=[w, s1], W=[W2])
            k.op(dve, lambda h: h.tensor_copy(out=bias2[:], in_=pb[:, 0:48]), R=[pb], W=[bias2])
            for vi, (vt, c0) in enumerate(((mod, 16), (s2, 0), (mod, 24), (mod, 40))):
                for half in range(2):
                    pv = nf()
                    for j in range(4):
                        kc = half * 4 + j
                        k.op(pool, lambda h: h.tensor_copy(out=vb_l[:], in_=vt[:, c0 + kc:c0 + kc + 1].to_broadcast([P, P])), R=[vt], W=[vb_l])
                        k.op(pe, lambda h: h.matmul(pv[:, j * P:(j + 1) * P], lhsT=vb_l[:], rhs=ident_f, start=True, stop=True), R=[vb_l, cst], W=[pv])
                    k.op(act, CP(act, vbc[:, vi, half * 512:(half + 1) * 512], pv[:]), R=[pv], W=[vbc])
            k.barrier()
        gtm_bc, s2_bc, shf_bc, gtf_bc = [vbc[:, i, :] for i in range(4)]

        xres = k.sb("xres", [P, TPG, D], F32, es)
        d_x = [k.dsem(f"x2{i}") for i in range(TPG)]
        d_xh = k.dsem("xh2")
        ssx = k.sb("ssx2", [P, 4], F32, es)
        hb = k.sb("hb2", [P, D], BF16, es)
        hT = k.sb("hT2", [P, KC, GW + 2], BF16, es)
        c_sb = k.sb("c_sb", [P, GW + 2], F32, es)
        cu = k.sb("cu", [P, GW + 2], F32, es)
        cv = k.sb("cv", [P, GW], F32, es)
        ybin = k.sb("ybin", [P, KC, GW], BF16, es)
        ogT = k.sb("ogT", [P, KC, GW], BF16, es)
        mrg = k.sb("mrg", [P, KC, GW], BF16, es)
        sga = k.sb("sga", [P, GW], F32, es)
        sgb = k.sb("sgb", [P, GW], F32, es)
        t1 = k.sb("t1", [P, GW], F32, es)
        t2 = k.sb("t2", [P, GW], F32, es)
        of_ = k.sb("of_", [P, NH, P], F32, es)
        x1v = of_[:].rearrange("p a b -> p (a b)")
        xhv = of_[0:2, :, :].rearrange("p a b -> p (a b)")
        d_g = k.dsem("g2")
        osq = k.sb("osq", [P, NH, P], F32, es)
        h2v = osq[:].rearrange("p a b -> p (a b)")
        oss = k.sb("oss", [P, NH], F32, es)
        zs = k.sb("zs", [P, D], F32, es)
        h2Tv = zs[:].rearrange("p (a b) -> p a b", a=KC)
        obv = zs[:].rearrange("p (a b) -> p a b", a=NH)
        d_s = k.dsem("s2")
        d_sc = k.dsem("sc2")
        d_zf = k.dsem("zf2")
        lg = k.sb("lg", [P, NE], F32, es)
        m8 = k.sb("m8", [P, 8], F32, es)
        rt = k.sb("rt", [P, 8, NE], F32, es)
        r1 = k.sb("r1c", [P, 8], F32, es)
        dkf = k.sb("dkf", [P, 4], F32, es)

        k.op(pool, lambda h: h.memset(hb[:], 0.0), W=[hb])
        xbv = xbuf[:, :].rearrange("(n p) d -> n p d", p=P)
        for n_ in range(NSLOT // P):
            k.dma(sp, lambda h: h.dma_start(out=xbv[n_], in_=hb[:]), d_zf, R=[hb], W=[xbuf])
        for g in range(NGQ):
            t0 = g * GW
            for ti in range(TPG):
                k.dma(sp, lambda h: h.dma_start(out=xres[:, ti, :], in_=xo[1 + t0 + ti * P:1 + t0 + (ti + 1) * P, :]), d_x[ti], W=[xres])
            k.dma(sp, lambda h: h.dma_start(out=xhv[0:1, :], in_=xo[t0:t0 + 1, :]), d_xh, W=[of_])
            k.dma(sp, lambda h: h.dma_start(out=xhv[1:2, :], in_=xo[t0 + GW + 1:t0 + GW + 2, :]), d_xh, W=[of_])
            for ti in range(TPG):
                k.op(act, lambda h: h.activation(out=zs[:], in_=xres[:, ti, :], func=AF.Square), R=[xres], W=[zs])
                k.op(dve, lambda h: h.tensor_reduce(out=ssx[:, 0:1], in_=zs[:], axis=AX.X, op=ALU.add), R=[zs], W=[ssx])
                rsqrt_(ssx, ssx[:, 0:1], EPS, 1.0 / D)
                k.op(act, lambda h: h.activation(out=hb[:], in_=xres[:, ti, :], func=AF.Copy, scale=ssx[:, 0:1]), R=[xres, ssx], W=[hb])
                for half in range(2):
                    pt, s = nb()
                    for j in range(4):
                        kc = half * 4 + j
                        k.op(pe, lambda h: h.transpose(out=pt[:, s, j, :], in_=hb[:, kc * P:(kc + 1) * P], identity=ident_b), R=[hb, cstb], W=[pt])
                    e_ = act if half else dve
                    k.op(e_, CP(e_, hT[:, half * 4:half * 4 + 4, 1 + ti * P:1 + (ti + 1) * P], pt[:, s, :, :]), R=[pt], W=[hT])
            k.op(act, lambda h: h.activation(out=zs[0:2, :], in_=xhv, func=AF.Square), R=[of_], W=[zs])
            k.op(dve, lambda h: h.tensor_reduce(out=ssx[0:2, 1:2], in_=zs[0:2, :], axis=AX.X, op=ALU.add), R=[zs], W=[ssx])
            rsqrt_(ssx, ssx[0:2, 1:2], EPS, 1.0 / D)
            k.op(act, lambda h: h.activation(out=hb[0:2, :], in_=xhv, func=AF.Copy, scale=ssx[0:2, 1:2]), R=[of_, ssx], W=[hb])
            for half in range(2):
                pt, s = nb()
                for j in range(4):
                    kc = half * 4 + j
                    k.op(pe, lambda h: h.transpose(out=pt[:, s, j, 0:2], in_=hb[0:2, kc * P:(kc + 1) * P], identity=cstb[0:2, 0, 0:2]), R=[hb, cstb], W=[pt])
                k.op(dve, lambda h: h.tensor_copy(out=hT[:, half * 4:half * 4 + 4, 0:1], in_=pt[:, s, :, 0:1]), R=[pt], W=[hT])
                k.op(dve, lambda h: h.tensor_copy(out=hT[:, half * 4:half * 4 + 4, GW + 1:GW + 2], in_=pt[:, s, :, 1:2]), R=[pt], W=[hT])

            def proj(col0, n0, n1):
                pm = nf()
                for kc in range(KC):
                    k.op(pe, lambda h: h.matmul(pm[:, 0:n1 - n0], lhsT=W2[:, kc, col0:col0 + P], rhs=hT[:, kc, n0:n1], start=(kc == 0), stop=(kc == KC - 1)),
                         R=[W2, hT], W=[pm])
                return pm

            for j in range(8):
                bc_, bu_, bb_ = bias2[:, 16 + j:17 + j], bias2[:, 24 + j:25 + j], bias2[:, 8 + j:9 + j]
                pc = proj(2 * D + j * P, 1, GW + 1)
                pch = proj(2 * D + j * P, 0, 1)
                pch2 = proj(2 * D + j * P, GW + 1, GW + 2)
                k.op(act, lambda h: h.activation(out=c_sb[:, 1:GW + 1], in_=pc[:, 0:GW], func=AF.Identity, bias=bc_), R=[pc, bias2], W=[c_sb])
                k.op(act, lambda h: h.activation(out=c_sb[:, 0:1], in_=pch[:, 0:1], func=AF.Identity, bias=bc_), R=[pch, bias2], W=[c_sb])
                k.op(act, lambda h: h.activation(out=c_sb[:, GW + 1:GW + 2], in_=pch2[:, 0:1], func=AF.Identity, bias=bc_), R=[pch2, bias2], W=[c_sb])
                pu = proj(3 * D + j * P, 1, GW + 1)
                puh = proj(3 * D + j * P, 0, 1)
                puh2 = proj(3 * D + j * P, GW + 1, GW + 2)
                k.op(dve, lambda h: h.scalar_tensor_tensor(out=cu[:, 1:GW + 1], in0=pu[:, 0:GW], scalar=bu_, in1=c_sb[:, 1:GW + 1], op0=ALU.add, op1=ALU.mult),
                     R=[pu, bias2, c_sb], W=[cu])
                k.op(dve, lambda h: h.scalar_tensor_tensor(out=cu[:, 0:1], in0=puh[:, 0:1], scalar=bu_, in1=c_sb[:, 0:1], op0=ALU.add, op1=ALU.mult),
                     R=[puh, bias2, c_sb], W=[cu])
                k.op(dve, lambda h: h.scalar_tensor_tensor(out=cu[:, GW + 1:GW + 2], in0=puh2[:, 0:1], scalar=bu_, in1=c_sb[:, GW + 1:GW + 2], op0=ALU.add, op1=ALU.mult),
                     R=[puh2, bias2, c_sb], W=[cu])
                if g == 0:
                    k.op(dve, lambda h: h.tensor_scalar(out=cu[:, 0:1], in0=cu[:, 0:1], scalar1=hm[:, 0:1], scalar2=None, op0=ALU.mult), R=[cu, hm], W=[cu])
                if g == NGQ - 1:
                    k.op(dve, lambda h: h.tensor_scalar(out=cu[:, GW + 1:GW + 2], in0=cu[:, GW + 1:GW + 2], scalar1=hm[:, 1:2], scalar2=None, op0=ALU.mult),
                         R=[cu, hm], W=[cu])
                k.op(dve, lambda h: h.tensor_scalar(out=cv[:], in0=cu[:, 0:GW], scalar1=cs3[:, j, 0:1], scalar2=None, op0=ALU.mult), R=[cu, cs3], W=[cv])
                k.op(dve, lambda h: h.scalar_tensor_tensor(out=cv[:], in0=cu[:, 1:GW + 1], scalar=cs3[:, j, 1:2], in1=cv[:], op0=ALU.mult, op1=ALU.add),
                     R=[cu, cs3, cv], W=[cv])
                k.op(dve, lambda h: h.scalar_tensor_tensor(out=cv[:], in0=cu[:, 2:GW + 2], scalar=cs3[:, j, 2:3], in1=cv[:], op0=ALU.mult, op1=ALU.add),
                     R=[cu, cs3, cv], W=[cv])
                pbm = proj(D + j * P, 1, GW + 1)
                k.op(dve, lambda h: h.scalar_tensor_tensor(out=ybin[:, j, :], in0=pbm[:, 0:GW], scalar=bb_, in1=cv[:], op0=ALU.add, op1=ALU.mult),
                     R=[pbm, bias2, cv], W=[ybin])

            for ti in range(TPG):
                it = g * TPG + ti
                k.dma(pool, lambda h: h.indirect_dma_start(out=of_[:].rearrange("p a b -> p (a b)"), out_offset=None, in_=o_dram[0][:, :],
                                                           in_offset=bass.IndirectOffsetOnAxis(ap=idx[:, 0, it:it + 1], axis=0),
                                                           bounds_check=bc_o, oob_is_err=False), d_g, R=[o_dram[0], idx], W=[of_])
                k.dma(pool, lambda h: h.indirect_dma_start(out=zs[:], out_offset=None, in_=o_dram[1][:, :],
                                                           in_offset=bass.IndirectOffsetOnAxis(ap=idx[:, 1, it:it + 1], axis=0),
                                                           bounds_check=bc_o, oob_is_err=False), d_g, R=[o_dram[1], idx], W=[zs])
                k.op(pool, lambda h: h.tensor_tensor(out=of_[:], in0=of_[:], in1=obv, op=ALU.add), R=[of_, zs], W=[of_])
                k.op(pool, lambda h: h.tensor_tensor(out=osq[:], in0=of_[:], in1=of_[:], op=ALU.mult), R=[of_], W=[osq])
                k.op(dve, lambda h: h.tensor_reduce(out=oss[:], in_=osq[:], axis=AX.X, op=ALU.add), R=[osq], W=[oss])
                rsqrt_(oss, oss[:], P * EPS)
                k.op(dve, lambda h: h.tensor_tensor(out=osq[:], in0=of_[:], in1=bc_h(oss[:, :], NH, P), op=ALU.mult), R=[of_, oss], W=[osq])
                k.op(pool, lambda h: h.tensor_tensor(out=osq[:], in0=osq[:], in1=on_bc[:].unsqueeze(1).to_broadcast([P, NH, P]), op=ALU.mult), R=[osq, on_bc], W=[osq])
                for half in range(2):
                    pz_ = nf()
                    for kc in range(KC):
                        k.op(pe, lambda h: h.matmul(pz_[:], lhsT=hT[:, kc, 1 + ti * P:1 + (ti + 1) * P], rhs=W2[:, kc, half * 512:(half + 1) * 512],
                                                    start=(kc == 0), stop=(kc == KC - 1)), R=[hT, W2], W=[pz_])
                    k.op(dve, lambda h: h.tensor_tensor(out=zs[:, half * 512:(half + 1) * 512], in0=pz_[:], in1=bz_bc[:, half * 512:(half + 1) * 512], op=ALU.add),
                         R=[pz_, bz_bc], W=[zs])
                k.op(act, lambda h: h.activation(out=zs[:], in_=zs[:], func=AF.Silu), R=[zs], W=[zs])
                k.op(dve, lambda h: h.scalar_tensor_tensor(out=hb[:], in0=osq[:].rearrange("p a b -> p (a b)"), scalar=float(P) ** 0.5, in1=zs[:], op0=ALU.mult, op1=ALU.mult),
                     R=[osq, zs], W=[hb])
                for half in range(2):
                    pt, s = nb()
                    for j in range(4):
                        kc = half * 4 + j
                        k.op(pe, lambda h: h.transpose(out=pt[:, s, j, :], in_=hb[:, kc * P:(kc + 1) * P], identity=ident_b), R=[hb, cstb], W=[pt])
                    e_ = act if half else dve
                    k.op(e_, CP(e_, ogT[:, half * 4:half * 4 + 4, ti * P:(ti + 1) * P], pt[:, s, :, :]), R=[pt], W=[ogT])

            for n in range(8):
                pga = proj(4 * D + n * P, 1, GW + 1)
                k.op(act, lambda h: h.activation(out=sga[:], in_=pga[:, 0:GW], func=AF.Sigmoid, bias=bias2[:, 32 + n:33 + n]), R=[pga, bias2], W=[sga])
                pgb = proj(5 * D + n * P, 1, GW + 1)
                k.op(act, lambda h: h.activation(out=sgb[:], in_=pgb[:, 0:GW], func=AF.Sigmoid, bias=bias2[:, 40 + n:41 + n]), R=[pgb, bias2], W=[sgb])
                pya, pyb = nf(), nf()
                for kc in range(KC):
                    k.op(pe, lambda h: h.matmul(pya[:, 0:GW], lhsT=Wp[0][:, kc, n * P:(n + 1) * P], rhs=ogT[:, kc, :], start=(kc == 0), stop=(kc == KC - 1)),
                         R=[Wp[0], ogT], W=[pya])
                for kc in range(KC):
                    k.op(pe, lambda h: h.matmul(pyb[:, 0:GW], lhsT=Wp[1][:, kc, n * P:(n + 1) * P], rhs=ybin[:, kc, :], start=(kc == 0), stop=(kc == KC - 1)),
                         R=[Wp[1], ybin], W=[pyb])
                k.op(dve, lambda h: h.tensor_tensor(out=t1[:], in0=pya[:, 0:GW], in1=sga[:], op=ALU.mult), R=[pya, sga], W=[t1])
                k.op(dve, lambda h: h.tensor_tensor(out=t2[:], in0=pyb[:, 0:GW], in1=sgb[:], op=ALU.mult), R=[pyb, sgb], W=[t2])
                k.op(pool, lambda h: h.tensor_tensor(out=mrg[:, n, :], in0=t1[:], in1=t2[:], op=ALU.add), R=[t1, t2], W=[mrg])

            for ti in range(TPG):
                it = g * TPG + ti
                for half in range(2):
                    pm = nf()
                    for kc in range(KC):
                        k.op(pe, lambda h: h.matmul(pm[:], lhsT=mrg[:, kc, ti * P:(ti + 1) * P], rhs=Wp[2][:, kc, half * 512:(half + 1) * 512],
                                                    start=(kc == 0), stop=(kc == KC - 1)), R=[mrg, Wp[2]], W=[pm])
                    hs_ = slice(half * 512, (half + 1) * 512)
                    k.op(dve, lambda h: h.tensor_tensor(out=x1v[:, hs_], in0=pm[:], in1=gtm_bc[:, hs_], op=ALU.mult), R=[pm, vbc], W=[of_])
                k.op(pool, lambda h: h.tensor_tensor(out=x1v, in0=x1v, in1=xres[:, ti, :], op=ALU.add), R=[of_, xres], W=[of_])
                k.dma(sp, lambda h: h.dma_start(out=x1_d[it * P:(it + 1) * P, :], in_=x1v), d_s, R=[of_], W=[x1_d])
                k.op(act, lambda h: h.activation(out=h2v, in_=x1v, func=AF.Square), R=[of_], W=[osq])
                k.op(dve, lambda h: h.tensor_reduce(out=ssx[:, 2:3], in_=h2v, axis=AX.X, op=ALU.add), R=[osq], W=[ssx])
                rsqrt_(ssx, ssx[:, 2:3], D * EPS)
                k.op(dve, lambda h: h.scalar_tensor_tensor(out=h2v, in0=x1v, scalar=ssx[:, 2:3], in1=s2_bc, op0=ALU.mult, op1=ALU.mult), R=[of_, ssx, vbc], W=[osq])
                k.op(dve, lambda h: h.scalar_tensor_tensor(out=h2v, in0=h2v, scalar=float(D) ** 0.5, in1=shf_bc, op0=ALU.mult, op1=ALU.add), R=[osq, vbc], W=[osq])
                k.op(act, CP(act, hb[:], h2v), R=[osq], W=[hb])
                for half in range(2):
                    pt = nf()
                    for j in range(4):
                        kc = half * 4 + j
                        k.op(pe, lambda h: h.transpose(out=pt[:, j * P:(j + 1) * P], in_=h2v[:, kc * P:(kc + 1) * P], identity=ident_f), R=[osq, cst], W=[pt])
                    e_ = act if half else dve
                    k.op(e_, CP(e_, h2Tv[:, half * 4:half * 4 + 4, :], pt[:].rearrange("p (a b) -> p a b", a=4)), R=[pt], W=[zs])
                pl = nf()
                for kc in range(KC):
                    k.op(pe, lambda h: h.matmul(pl[:, 0:NE], lhsT=h2Tv[:, kc, :], rhs=rw[:, kc, :], start=(kc == 0), stop=(kc == KC - 1)), R=[zs, rw], W=[pl])
                k.op(dve, lambda h: h.tensor_tensor(out=lg[:], in0=pl[:, 0:NE], in1=rb_bc[:], op=ALU.add), R=[pl, rb_bc], W=[lg])
                k.op(dve, lambda h: h.max(out=m8[:], in_=lg[:]), R=[lg], W=[m8])
                sel, ex, G_, pos, val, dest, oh, tmp = [rt[:, i, :] for i in range(8)]
                k.op(dve, lambda h: h.tensor_scalar(out=sel, in0=lg[:], scalar1=m8[:, 3:4], scalar2=None, op0=ALU.is_ge), R=[lg, m8], W=[rt])
                k.op(dve, lambda h: h.tensor_scalar(out=r1[:, 0:1], in0=m8[:, 0:1], scalar1=-1.0, scalar2=None, op0=ALU.mult), R=[m8], W=[r1])
                k.op(act, lambda h: h.activation(out=ex, in_=lg[:], func=AF.Exp, bias=r1[:, 0:1]), R=[lg, r1], W=[rt])
                k.op(dve, lambda h: h.tensor_tensor(out=ex, in0=ex, in1=sel, op=ALU.mult), R=[rt], W=[rt])
                k.op(dve, lambda h: h.tensor_reduce(out=r1[:, 1:2], in_=ex, axis=AX.X, op=ALU.add), R=[rt], W=[r1])
                k.op(dve, lambda h: h.reciprocal(out=r1[:, 2:3], in_=r1[:, 1:2]), R=[r1], W=[r1])
                k.op(dve, lambda h: h.tensor_scalar(out=G_, in0=ex, scalar1=r1[:, 2:3], scalar2=None, op0=ALU.mult), R=[rt, r1], W=[rt])
                k.op(pool, lambda h: h.tensor_copy(out=selb[:, it, :], in_=sel), R=[rt], W=[selb])
                pp = nf()
                for i2 in range(it):
                    k.op(pe, lambda h: h.matmul(pp[:, 0:NE], lhsT=ONES_b, rhs=selb[:, i2, :], start=(i2 == 0), stop=False), R=[cstb, selb], W=[pp])
                k.op(pe, lambda h: h.matmul(pp[:, 0:NE], lhsT=SUfull_b, rhs=selb[:, it, :], start=(it == 0), stop=True), R=[cstb, selb], W=[pp])
                k.op(dve, lambda h: h.tensor_copy(out=pos, in_=pp[:, 0:NE]), R=[pp], W=[rt])
                k.op(dve, lambda h: h.tensor_scalar(out=val, in0=pos, scalar1=float(CAP) - 0.5, scalar2=None, op0=ALU.is_lt), R=[rt], W=[rt])
                k.op(dve, lambda h: h.tensor_tensor(out=val, in0=val, in1=sel, op=ALU.mult), R=[rt], W=[rt])
                k.op(dve, lambda h: h.tensor_tensor(out=dest, in0=pos, in1=ec_bc[:], op=ALU.add), R=[rt, ec_bc], W=[rt])
                k.op(dve, lambda h: h.scalar_tensor_tensor(out=dest, in0=dest, scalar=-BIG, in1=val, op0=ALU.add, op1=ALU.mult), R=[rt], W=[rt])
                k.op(dve, lambda h: h.tensor_scalar(out=dest, in0=dest, scalar1=BIG, scalar2=None, op0=ALU.add), R=[rt], W=[rt])
                for kk in range(4):
                    k.op(dve, lambda h: h.tensor_scalar(out=oh, in0=lg[:], scalar1=m8[:, kk:kk + 1], scalar2=None, op0=ALU.is_equal), R=[lg, m8], W=[rt])
                    k.op(dve, lambda h: h.tensor_tensor(out=tmp, in0=oh, in1=dest, op=ALU.mult), R=[rt], W=[rt])
                    k.op(dve, lambda h: h.tensor_reduce(out=dkf[:, kk:kk + 1], in_=tmp, axis=AX.X, op=ALU.add), R=[rt], W=[dkf])
                    k.op(dve, lambda h: h.tensor_tensor(out=tmp, in0=oh, in1=G_, op=ALU.mult), R=[rt], W=[rt])
                    k.op(dve, lambda h: h.tensor_reduce(out=gk[:, it, kk:kk + 1], in_=tmp, axis=AX.X, op=ALU.add), R=[rt], W=[gk])
                k.op(dve, lambda h: h.tensor_copy(out=dki[:, it, :], in_=dkf[:]), R=[dkf], W=[dki])
                for kk in range(4):
                    k.dma(pool, lambda h: h.indirect_dma_start(out=xbuf[:, :], out_offset=bass.IndirectOffsetOnAxis(ap=dki[:, it, kk:kk + 1], axis=0),
                                                               in_=hb[:, :], in_offset=None, bounds_check=bc_s, oob_is_err=False),
                          d_sc, R=[hb, dki], W=[xbuf])
        k.barrier()

    if stop < 4:
        return
    with ExitStack() as es:
        w1 = [k.sb(f"w1_{i}", [P, KC, 2 * D], BF16, es) for i in range(2)]
        w2 = [k.sb(f"w2_{i}", [P, KC, D], BF16, es) for i in range(2)]
        d_w1 = [k.dsem(f"mw1{i}") for i in range(2)]
        d_w2 = [k.dsem(f"mw2{i}") for i in range(2)]
        b2bc = [k.sb(f"b2bc{i}", [P, D], F32, es) for i in range(2)]
        d_b2 = [k.dsem(f"mb2{i}") for i in range(2)]
        b1 = k.sb("b1", [P, NE, 16], F32, es)
        xe = [k.sb(f"xe{i}", [P, CT, D], BF16, es) for i in range(2)]
        d_xe = [k.dsem(f"xe{i}") for i in range(2)]
        xeT = k.sb("xeT", [P, KC, CAP], BF16, es)
        actT = k.sb("actT", [P, KC, CAP], BF16, es)
        NSC = -(-CAP // 384)
        CW = CAP // NSC
        g1 = [k.sb(f"g1_{i}", [P, CW], F32, es) for i in range(2)]
        sg = [k.sb(f"sg_{i}", [P, CW], F32, es) for i in range(2)]
        u1 = [k.sb(f"u1_{i}", [P, CW], F32, es) for i in range(2)]
        ysb = [k.sb(f"ysb{i}", [P, D], F32, es) for i in range(2)]
        d_y = [k.dsem(f"y{i}") for i in range(2)]
        psf = [k.ps(f"p3f{i}", [P, 512], F32, es) for i in range(6)]
        psb_t = [k.ps(f"p3b{i}", [P, 2, 4, P], BF16, es) for i in range(2)]
        rr = {"f": 0, "b": 0}

        def nf():
            rr["f"] += 1
            return psf[rr["f"] % 6]

        def nb():
            rr["b"] += 1
            return psb_t[(rr["b"] // 2) % 2], rr["b"] % 2
        k.dma(sp, lambda h: h.dma_start(out=b1[:], in_=moe_b1_fm), None, W=[b1])

        def load_w(e):
            z = e % 2
            k.dma(pool, lambda h: h.dma_start(out=w1[z][:], in_=moe_w1[e].rearrange("(k p) f -> p k f", p=P)), d_w1[z], W=[w1[z]])
            k.dma(pool, lambda h: h.dma_start(out=w2[z][:], in_=moe_w2[e].rearrange("(k p) f -> p k f", p=P)), d_w2[z], W=[w2[z]])
            k.dma(sp, lambda h: h.dma_start(out=b2bc[z][:], in_=moe_b2[e:e + 1, :].partition_broadcast(P)), d_b2[z], W=[b2bc[z]])
            k.dma(sp, lambda h: h.dma_start(out=xe[z][:], in_=xbuf[e * CAP:(e + 1) * CAP, :].rearrange("(c p) d -> p c d", p=P)), d_xe[z], R=[xbuf], W=[xe[z]])
        load_w(0)
        yi = 0
        zc = 0
        for e in range(NE):
            z = e % 2
            if e + 1 < NE:
                load_w(e + 1)
            for st in range(CT):
                for half in range(2):
                    pt, s = nb()
                    for j in range(4):
                        kc = half * 4 + j
                        k.op(pe, lambda h: h.transpose(out=pt[:, s, j, :], in_=xe[z][:, st, kc * P:(kc + 1) * P], identity=ident_b), R=[xe[z], cstb], W=[pt])
                    e_ = act if half else dve
                    k.op(e_, CP(e_, xeT[:, half * 4:half * 4 + 4, st * P:(st + 1) * P], pt[:, s, :, :]), R=[pt], W=[xeT])
            for fc in range(8):
                for sc_ in range(NSC):
                    zc += 1
                    zz = zc % 2
                    cs_ = slice(sc_ * CW, (sc_ + 1) * CW)
                    pg_, pu_ = nf(), nf()
                    for kc in range(KC):
                        k.op(pe, lambda h: h.matmul(pg_[:, 0:CW], lhsT=w1[z][:, kc, fc * P:(fc + 1) * P], rhs=xeT[:, kc, cs_], start=(kc == 0), stop=(kc == KC - 1)),
                             R=[w1[z], xeT], W=[pg_])
                    for kc in range(KC):
                        k.op(pe, lambda h: h.matmul(pu_[:, 0:CW], lhsT=w1[z][:, kc, D + fc * P:D + (fc + 1) * P], rhs=xeT[:, kc, cs_], start=(kc == 0), stop=(kc == KC - 1)),
                             R=[w1[z], xeT], W=[pu_])
                    k.op(dve, lambda h: h.tensor_scalar(out=g1[zz][:], in0=pg_[:, 0:CW], scalar1=b1[:, e, fc:fc + 1], scalar2=7.0, op0=ALU.add, op1=ALU.min),
                         R=[pg_, b1], W=[g1[zz]])
                    k.op(act, lambda h: h.activation(out=sg[zz][:], in_=g1[zz][:], func=AF.Sigmoid, scale=1.702), R=[g1[zz]], W=[sg[zz]])
                    k.op(dve, lambda h: h.tensor_scalar(out=u1[zz][:], in0=pu_[:, 0:CW], scalar1=b1[:, e, 8 + fc:9 + fc], scalar2=7.0, op0=ALU.add, op1=ALU.min),
                         R=[pu_, b1], W=[u1[zz]])
                    k.op(dve, lambda h: h.tensor_scalar(out=u1[zz][:], in0=u1[zz][:], scalar1=-7.0, scalar2=1.0, op0=ALU.max, op1=ALU.add), R=[u1[zz]], W=[u1[zz]])
                    k.op(dve, lambda h: h.tensor_tensor(out=g1[zz][:], in0=g1[zz][:], in1=sg[zz][:], op=ALU.mult), R=[g1[zz], sg[zz]], W=[g1[zz]])
                    k.op(dve, lambda h: h.tensor_tensor(out=actT[:, fc, cs_], in0=g1[zz][:], in1=u1[zz][:], op=ALU.mult), R=[g1[zz], u1[zz]], W=[actT])
            for st in range(CT):
                yi += 1
                Y = ysb[yi % 2]
                for half in range(2):
                    py = nf()
                    for fc in range(8):
                        k.op(pe, lambda h: h.matmul(py[:], lhsT=actT[:, fc, st * P:(st + 1) * P], rhs=w2[z][:, fc, half * 512:(half + 1) * 512],
                                                    start=(fc == 0), stop=(fc == 7)), R=[actT, w2[z]], W=[py])
                    hs_ = slice(half * 512, (half + 1) * 512)
                    k.op(dve, lambda h: h.tensor_tensor(out=Y[:, hs_], in0=py[:], in1=b2bc[z][:, hs_], op=ALU.add), R=[py, b2bc[z]], W=[Y])
                r0 = e * CAP + st * P
                k.dma(sp, lambda h: h.dma_start(out=ybuf[r0:r0 + P, :], in_=Y[:]), d_y[yi % 2], R=[Y], W=[ybuf])
        k.barrier()

    if stop < 5:
        return
    with ExitStack() as es:
        yk = [k.sb(f"yk{i}", [P, D], F32, es) for i in range(4)]
        fnw_bc = k.sb("fnw_bc", [P, D], F32, es)
        k.dma(sp, lambda h: h.dma_start(out=fnw_bc[:], in_=fnw.partition_broadcast(P)), None, W=[fnw_bc])
        d_yk = [k.dsem(f"yk{i}") for i in range(4)]
        x1t = k.sb("x1t", [P, D], F32, es)
        d_x1 = k.dsem("x1t")
        acc = k.sb("acc", [P, D], F32, es)
        junk = k.sb("junk3", [P, D], F32, es)
        ss3 = k.sb("ss3", [P, 1], F32, es)
        d_out = k.dsem("out")
        for i in range(4):
            k.op(pool, lambda h: h.memset(yk[i][:], 0.0), W=[yk[i]])
        for it in range(NTQ):
            k.dma(sp, lambda h: h.dma_start(out=x1t[:], in_=x1_d[it * P:(it + 1) * P, :]), d_x1, R=[x1_d], W=[x1t])
            for kk in range(4):
                k.dma(pool, lambda h: h.indirect_dma_start(out=yk[kk][:, :], out_offset=None, in_=ybuf[:, :],
                                                           in_offset=bass.IndirectOffsetOnAxis(ap=dki[:, it, kk:kk + 1], axis=0),
                                                           bounds_check=bc_s, oob_is_err=False), d_yk[kk], R=[ybuf, dki], W=[yk[kk]])
            k.op(dve, lambda h: h.tensor_scalar(out=acc[:], in0=yk[0][:], scalar1=gk[:, it, 0:1], scalar2=None, op0=ALU.mult), R=[yk[0], gk], W=[acc])
            for kk in range(1, 4):
                k.op(dve, lambda h: h.scalar_tensor_tensor(out=acc[:], in0=yk[kk][:], scalar=gk[:, it, kk:kk + 1], in1=acc[:], op0=ALU.mult, op1=ALU.add),
                     R=[yk[kk], gk, acc], W=[acc])
            k.op(pool, lambda h: h.tensor_tensor(out=acc[:], in0=acc[:], in1=vbc[:, 3, :], op=ALU.mult), R=[acc, vbc], W=[acc])
            k.op(pool, lambda h: h.tensor_tensor(out=acc[:], in0=acc[:], in1=x1t[:], op=ALU.add), R=[acc, x1t], W=[acc])
            k.op(act, lambda h: h.activation(out=junk[:], in_=acc[:], func=AF.Square), R=[acc], W=[junk])
            k.op(dve, lambda h: h.tensor_reduce(out=ss3[:], in_=junk[:], axis=AX.X, op=ALU.add), R=[junk], W=[ss3])
            rsqrt_(ss3, ss3[:], D * EPS)
            k.op(dve, lambda h: h.scalar_tensor_tensor(out=junk[:], in0=acc[:], scalar=ss3[:, 0:1], in1=fnw_bc[:], op0=ALU.mult, op1=ALU.mult), R=[acc, ss3, fnw_bc], W=[junk])
            k.op(pool, lambda h: h.tensor_scalar(out=junk[:], in0=junk[:], scalar1=float(D) ** 0.5, scalar2=None, op0=ALU.mult), R=[junk], W=[junk])
            k.dma(sp, lambda h: h.dma_start(out=out[it * P:(it + 1) * P, :], in_=junk[:]), d_out, R=[junk], W=[out])
        k.barrier()


def _fm(v, n):
    return np.ascontiguousarray(np.asarray(v, np.float32).reshape(n, P).T)


def prep(inp, TSEQ):
    g = {k_: np.asarray(v) for k_, v in inp.items()}
    w_in = g["w_in"][0]
    cuts = np.cumsum([3 * D, D, 16, 16, 3 * D, 2 * D])
    w_qkv = np.ascontiguousarray(w_in[:, :cuts[0]])
    w_z = np.ascontiguousarray(w_in[:, cuts[0]:cuts[1]])
    w_b = w_in[:, cuts[1]:cuts[2]]
    w_a = w_in[:, cuts[2]:cuts[3]]
    w_sc = np.ascontiguousarray(w_in[:, cuts[3]:cuts[4]])
    w_g = np.ascontiguousarray(w_in[:, cuts[4]:cuts[5]])
    w_bd = np.ascontiguousarray(np.concatenate([w_b[:, 0:8], w_a[:, 0:8], w_b[:, 8:16], w_a[:, 8:16]], axis=1))
    negA_dt = np.zeros((1, 64), np.float32)
    negA_dt[0, 0:16] = g["a_log"][0].reshape(16)
    negA_dt[0, 16:32] = g["dt_bias"][0].reshape(16)
    convq_fm = np.ascontiguousarray(g["conv_qkv_w"][0].T.reshape(24, P, 5).transpose(1, 0, 2))
    _, consts = _consts()
    shared = dict(ada_w=np.ascontiguousarray(g["ada_w"][0]), ada_b_fm=_fm(g["ada_b"][0], 48), n1_fm=_fm(g["norm1_w"][0], 8),
                  w_qkv=w_qkv, w_bd=w_bd, convq_fm=convq_fm, negA_dt=negA_dt, consts=np.ascontiguousarray(consts))
    TQ = TSEQ // 4
    NTQ = TQ // P
    CAP = P * int(np.ceil(CAPF * TQ / 8 / P))
    shared.update(
        w_z=w_z, w_sc=w_sc, w_g=w_g,
        convs_fm=np.ascontiguousarray(g["conv_sc_w"][0].T.reshape(8, P, 3).transpose(1, 0, 2)),
        onorm=np.ascontiguousarray(g["onorm_w"][0].reshape(1, P)),
        w_up_a=np.ascontiguousarray(g["w_up_a"][0]), w_out_sc=np.ascontiguousarray(g["w_out_sc"][0]), w_o=np.ascontiguousarray(g["w_o"][0]),
        n2_fm=_fm(g["norm2_w"][0], 8), router_w=np.ascontiguousarray(g["router_w"][0]),
        router_b=np.ascontiguousarray(g["router_b"][0].reshape(1, NE)),
        ecap=(np.arange(NE, dtype=np.float32) * CAP).reshape(1, NE),
        fnw=np.ascontiguousarray(g["final_norm_w"].reshape(1, D)),
        moe_w1=np.ascontiguousarray(g["moe_w1"][0]), moe_w2=np.ascontiguousarray(g["moe_w2"][0]),
        moe_b1_fm=np.ascontiguousarray(g["moe_b1"][0].reshape(NE, 16, P).transpose(2, 0, 1)),
        moe_b2=np.ascontiguousarray(g["moe_b2"][0]))
    maps = []
    pp = np.arange(P)[:, None]
    for c in range(8):
        b, q = c // 4, c % 4
        xb = np.ascontiguousarray(g["x"][b])
        m = dict(shared)
        m["xf"] = xb
        m["xr"] = np.ascontiguousarray(xb[::-1])
        m["c_fm"] = _fm(g["c"][b], 8)
        xo = np.zeros((TQ + 2, D), np.float32)
        lo, hi = q * TQ - 1, (q + 1) * TQ + 1
        xo[max(0, -lo):TQ + 2 - max(0, hi - TSEQ)] = xb[max(lo, 0):min(hi, TSEQ)]
        m["xo"] = xo
        m["hmask"] = np.repeat(np.array([[float(lo >= 0), float(hi <= TSEQ)]], np.float32), P, 0)
        tok = q * TQ + np.arange(NTQ)[None, :] * P + pp
        m["idx"] = np.ascontiguousarray(np.stack([tok, TSEQ - 1 - tok], axis=1).astype(np.int32))
        maps.append(m)
    return maps


_NC_CACHE = {}


def kernel(**inputs):
    TSEQ = int(np.asarray(inputs["x"]).shape[1])
    import os
    stop = int(os.environ.get("KSTOP", "99"))
    if TSEQ not in _NC_CACHE:
        _NC_CACHE[TSEQ] = build(TSEQ, stop=stop)
    maps = prep(inputs, TSEQ)
    res = run_bass_kernel_spmd(_NC_CACHE[TSEQ], maps, core_ids=list(range(8)))
    TQ = TSEQ // 4
    out = np.empty((2, TSEQ, D), np.float32)
    for c in range(8):
        out[c // 4, (c % 4) * TQ:(c % 4 + 1) * TQ] = res.results[c]["out"] if "out" in res.results[c] else 0.0
    return out
```

```python
import numpy as np
from contextlib import ExitStack
import concourse.bass as bass
import concourse.mybir as mybir
from concourse.bass_utils import run_bass_kernel_spmd

F32 = mybir.dt.float32
BF16 = mybir.dt.bfloat16
I32 = mybir.dt.int32
U32 = mybir.dt.uint32
AF = mybir.ActivationFunctionType
ALU = mybir.AluOpType
AX = mybir.AxisListType

P = 128
D = 1024
KC = 8
NH = 8
NE = 32
EPS = 1e-6
CAPF = 3.0


class Sem:
    def __init__(self, h):
        self.h = h
        self.n = 0
        self.is_dma = False


class Tl:
    def __init__(self, t):
        self.t = t
        self.w = {}
        self.r = {}

    def __getitem__(self, k):
        return self.t[k]


class Eng:
    def __init__(self, h, sem, same):
        self.h = h
        self.sem = sem
        self.seen = {}
        self.same = same


class K:
    def __init__(self, nc):
        self.nc = nc
        self.es = ExitStack()
        self.nsem = 0
        mk = lambda h, nm, same: Eng(h, self.newsem(nm), same)
        self.pe = mk(nc.tensor, "pe", False)
        self.act = mk(nc.scalar, "act", True)
        self.dve = mk(nc.vector, "dve", True)
        self.pool = mk(nc.gpsimd, "pool", True)
        self.sp = mk(nc.sync, "sp", False)
        self.engs = [self.pe, self.act, self.dve, self.pool, self.sp]
        self.esems = [e.sem for e in self.engs[:4]]
        self.dsems = []

    def newsem(self, name):
        self.nsem += 1
        return Sem(self.es.enter_context(self.nc.semaphore(f"{name}_{self.nsem}")))

    def dsem(self, name):
        s = self.newsem("d" + name)
        s.is_dma = True
        self.dsems.append(s)
        return s

    def sb(self, name, shape, dt, es=None):
        return Tl((es or self.es).enter_context(self.nc.sbuf_tensor(name, list(shape), dt)))

    def ps(self, name, shape, dt, es=None):
        return Tl((es or self.es).enter_context(self.nc.psum_tensor(name, list(shape), dt)))

    def _deps(self, eng, R, W):
        deps = {}

        def need(d):
            for s, v in d.items():
                if v > deps.get(s, 0):
                    deps[s] = v
        for t in R:
            need(t.w)
        for t in W:
            need(t.w)
            need(t.r)
        for s, v in deps.items():
            if s.is_dma:
                v = s.n
            if s is eng.sem and not eng.same:
                continue
            if eng.seen.get(s, 0) >= v:
                continue
            eng.h.wait_ge(s.h, v)
            eng.seen[s] = v

    def _stamp(self, s, R, W):
        for t in R:
            t.r[s] = s.n
        for t in W:
            t.w[s] = s.n
            t.r = {}

    SEM_LIMIT = 12000

    def op(self, eng, fn, R=(), W=()):
        if eng.sem.n >= self.SEM_LIMIT:
            eng.sem = self.newsem("roll")
            self.esems.append(eng.sem)
        self._deps(eng, R, W)
        inst = fn(eng.h)
        eng.sem.n += 1
        inst.then_inc(eng.sem.h, 1)
        self._stamp(eng.sem, R, W)

    def dma(self, eng, fn, ds=None, R=(), W=()):
        if ds is None:
            ds = self.dsem("os")
        self._deps(eng, R, W)
        inst = fn(eng.h)
        ds.n += 16
        inst.then_inc(ds.h, 16)
        self._stamp(ds, R, W)

    def barrier(self):
        sems = self.esems + self.dsems
        for e in self.engs:
            for s in sems:
                if s.n > 0 and e.seen.get(s, 0) < s.n and not (s is e.sem and not e.same):
                    e.h.wait_ge(s.h, s.n)
                    e.seen[s] = s.n


def CP(eng, out, in_):
    if hasattr(eng.h, "tensor_copy"):
        return lambda h: h.tensor_copy(out=out, in_=in_)
    return lambda h: h.activation(out=out, in_=in_, func=AF.Copy)


def _consts():
    i = np.arange(P)
    same = (i[:, None] // 64) == (i[None, :] // 64)
    c = {}
    c["ident"] = np.eye(P, dtype=np.float32)
    c["U"] = (same & (i[:, None] <= i[None, :])).astype(np.float32)
    c["SUx"] = (same & (i[:, None] > i[None, :])).astype(np.float32)
    c["OC0"] = np.repeat((i < 64).astype(np.float32)[:, None], P, 1)
    c["OC1"] = np.repeat((i >= 64).astype(np.float32)[:, None], P, 1)
    c["SLneg"] = -(same & (i[:, None] > i[None, :])).astype(np.float32)
    c["SUneg"] = -(same & (i[:, None] < i[None, :])).astype(np.float32)
    c["SUinc"] = (same & (i[:, None] <= i[None, :])).astype(np.float32)
    c["SUfull"] = (i[:, None] < i[None, :]).astype(np.float32)
    c["ONES"] = np.ones((P, P), np.float32)
    names = ["ident", "U", "SUx", "OC0", "OC1", "SLneg", "SUneg", "SUinc", "SUfull", "ONES"]
    return names, np.stack([c[n] for n in names], axis=1)


def build(TSEQ, debug=False, stop=99):
    NT = TSEQ // P
    NG = NT // 4
    TQ = TSEQ // 4
    NTQ = TQ // P
    nc = bass.Bass("TRN2", target_bir_lowering=False)
    k = K(nc)
    pe, act, dve, pool, sp = k.pe, k.act, k.dve, k.pool, k.sp
    din = lambda n, s, d=F32: nc.dram_tensor(n, list(s), d, kind="ExternalInput").ap()
    xs = [din("xf", [TSEQ, D]), din("xr", [TSEQ, D])]
    c_fm = din("c_fm", [P, KC])
    ada_w = din("ada_w", [D, 6 * D])
    ada_b_fm = din("ada_b_fm", [P, 48])
    n1_fm = din("n1_fm", [P, KC])
    w_qkv = din("w_qkv", [D, 3 * D])
    w_bd = din("w_bd", [D, 32])
    convq_fm = din("convq_fm", [P, 24, 5])
    negA_dt = din("negA_dt", [1, 64])
    consts = din("consts", [P, 10, P])
    o_kind = "ExternalOutput" if debug else "Internal"
    o_dram = [Tl(nc.dram_tensor(f"o_d{d}", [TSEQ, D], F32, kind=o_kind).ap()) for d in range(2)]

    dbg = {}
    if debug:
        dbg["gt"] = Tl(nc.dram_tensor("dbg_gt", [P, 40], F32, kind="ExternalOutput").ap())
        dbg["eg"] = Tl(nc.dram_tensor("dbg_eg", [P, 32], F32, kind="ExternalOutput").ap())
        dbg["mod"] = Tl(nc.dram_tensor("dbg_mod", [P, 48], F32, kind="ExternalOutput").ap())
        dbg["biasq"] = Tl(nc.dram_tensor("dbg_biasq", [P, 24], F32, kind="ExternalOutput").ap())
        dbg["hT"] = Tl(nc.dram_tensor("dbg_hT", [P, KC, 516], BF16, kind="ExternalOutput").ap())
        dbg["ssx"] = Tl(nc.dram_tensor("dbg_ssx", [P, 1], F32, kind="ExternalOutput").ap())
        dbg["hb"] = Tl(nc.dram_tensor("dbg_hb", [P, D], BF16, kind="ExternalOutput").ap())
        dbg["xt"] = Tl(nc.dram_tensor("dbg_xt", [P, D], F32, kind="ExternalOutput").ap())
        dbg["sqs"] = Tl(nc.dram_tensor("dbg_sqs", [P, D], F32, kind="ExternalOutput").ap())
        dbg["qkv"] = Tl(nc.dram_tensor("dbg_qkv", [P, 24, 512], BF16, kind="ExternalOutput").ap())
    es0 = ExitStack()
    cst = k.sb("cst", [P, 10, P], F32)
    cstb = k.sb("cstb", [P, 10, P], BF16)
    d_c = k.dsem("c")
    k.dma(sp, lambda h: h.dma_start(out=cst[:], in_=consts), None, W=[cst])
    k.op(dve, lambda h: h.tensor_copy(out=cstb[:], in_=cst[:]), R=[cst], W=[cstb])
    ident_f, U_f, SUx_f, OC0_f, OC1_f, SLneg_f, SUneg_f, SUinc_f = [cst[:, i, :] for i in range(8)]
    ident_b = cstb[:, 0, :]

    mod = k.sb("mod", [P, 48], F32)
    s1 = k.sb("s1", [P, KC], F32)
    cact = k.sb("cact", [P, KC], F32)
    with ExitStack() as es:
        cf = k.sb("cf", [P, KC], F32, es)
        adab = k.sb("adab", [P, 48], F32, es)
        n1 = k.sb("n1", [P, KC], F32, es)
        aw = [k.sb(f"aw{i}", [P, KC, 768], F32, es) for i in range(2)]
        d_aw = [k.dsem(f"aw{i}") for i in range(2)]
        pmod = k.ps("pmod", [P, 512], F32, es)
        k.dma(sp, lambda h: h.dma_start(out=cf[:], in_=c_fm), None, W=[cf])
        k.dma(sp, lambda h: h.dma_start(out=adab[:], in_=ada_b_fm), None, W=[adab])
        k.dma(sp, lambda h: h.dma_start(out=n1[:], in_=n1_fm), None, W=[n1])
        k.op(act, lambda h: h.activation(out=cact[:], in_=cf[:], func=AF.Silu), R=[cf], W=[cact])
        for blk in range(8):
            a = aw[blk % 2]
            k.dma(sp, lambda h: h.dma_start(out=a[:], in_=ada_w[:, blk * 768:(blk + 1) * 768].rearrange("(k p) c -> p k c", p=P)), d_aw[blk % 2], W=[a])
            for j in range(6):
                nk = blk * 6 + j
                for kc in range(KC):
                    k.op(pe, lambda h: h.matmul(pmod[:, nk:nk + 1], lhsT=a[:, kc, j * P:(j + 1) * P], rhs=cact[:, kc:kc + 1],
                                                start=(kc == 0), stop=(kc == KC - 1)), R=[a, cact], W=[pmod])
        k.op(dve, lambda h: h.tensor_tensor(out=mod[:], in0=pmod[:, 0:48], in1=adab[:], op=ALU.add), R=[pmod, adab], W=[mod])
        k.op(dve, lambda h: h.scalar_tensor_tensor(out=s1[:], in0=mod[:, 8:16], scalar=1.0, in1=n1[:], op0=ALU.add, op1=ALU.mult),
             R=[mod, n1], W=[s1])
        k.barrier()
    sh_m = lambda kc: mod[:, kc:kc + 1]

    es1 = ExitStack()
    wq = k.sb("wq", [P, KC, 3 * D], BF16, es1)
    wbd = k.sb("wbd", [P, KC, 32], BF16, es1)
    bias_q = k.sb("bias_q", [P, 24], F32, es1)
    bias_bd = k.sb("bias_bd", [P, 32], F32, es1)
    cq = k.sb("cq", [P, 24, 5], F32, es1)
    bsum = k.sb("bsum", [P, 24], F32, es1)
    gpar = k.sb("gpar", [P, 64], F32, es1)
    with ExitStack() as es:
        wst = [k.sb(f"wst{i}", [P, 3 * D], F32, es) for i in range(2)]
        d_w = [k.dsem(f"w{i}") for i in range(2)]
        wbs = k.sb("wbs", [P, KC, 32], F32, es)
        shb = k.sb("shb", [P, KC, P], F32, es)
        pb = k.ps("pb", [P, 512], F32, es)
        pb2 = k.ps("pb2", [P, 512], F32, es)
        k.dma(sp, lambda h: h.dma_start(out=cq[:], in_=convq_fm), None, W=[cq])
        k.dma(sp, lambda h: h.dma_start(out=gpar[:], in_=negA_dt.partition_broadcast(P)), None, W=[gpar])
        k.dma(sp, lambda h: h.dma_start(out=wbs[:], in_=w_bd.rearrange("(k p) c -> p k c", p=P)), None, W=[wbs])
        k.op(act, lambda h: h.activation(out=gpar[:, 0:16], in_=gpar[:, 0:16], func=AF.Exp), R=[gpar], W=[gpar])
        k.op(dve, lambda h: h.tensor_scalar(out=gpar[:, 0:16], in0=gpar[:, 0:16], scalar1=-1.0, scalar2=None, op0=ALU.mult),
             R=[gpar], W=[gpar])
        for kc in range(KC):
            k.op(pool, lambda h: h.tensor_copy(out=shb[:, kc, :], in_=mod[:, kc:kc + 1].to_broadcast([P, P])), R=[mod], W=[shb])
        for kc in range(KC):
            w = wst[kc % 2]
            k.dma(sp, lambda h: h.dma_start(out=w[:], in_=w_qkv[kc * P:(kc + 1) * P, :]), d_w[kc % 2], W=[w])
            k.op(dve if kc % 2 else pool, lambda h: h.tensor_scalar(out=wq[:, kc, :], in0=w[:], scalar1=s1[:, kc:kc + 1], scalar2=None,
                                                                    op0=ALU.mult), R=[w, s1], W=[wq])
            k.op(pe, lambda h: h.matmul(pb2[:, 0:32], lhsT=shb[:, kc, :], rhs=wbs[:, kc, :], start=(kc == 0), stop=(kc == KC - 1)),
                 R=[shb, wbs], W=[pb2])
            k.op(dve, lambda h: h.tensor_scalar(out=wbd[:, kc, :], in0=wbs[:, kc, :], scalar1=s1[:, kc:kc + 1], scalar2=None, op0=ALU.mult),
                 R=[wbs, s1], W=[wbd])
        for blk in range(8):
            w = wst[blk % 2]
            wv = w[:].rearrange("p (k c) -> p k c", k=KC)
            k.dma(sp, lambda h: h.dma_start(out=wv, in_=w_qkv[:, blk * 384:(blk + 1) * 384].rearrange("(k p) c -> p k c", p=P)), d_w[blk % 2], W=[w])
            for j in range(3):
                cc = blk * 3 + j
                for kc in range(KC):
                    k.op(pe, lambda h: h.matmul(pb[:, cc:cc + 1], lhsT=wv[:, kc, j * P:(j + 1) * P], rhs=sh_m(kc),
                                                start=(kc == 0), stop=(kc == KC - 1)), R=[w, mod], W=[pb])
        k.op(dve, lambda h: h.tensor_copy(out=bias_q[:], in_=pb[:, 0:24]), R=[pb], W=[bias_q])
        k.op(dve, lambda h: h.tensor_copy(out=bias_bd[:], in_=pb2[:, 0:32]), R=[pb2], W=[bias_bd])
        k.op(dve, lambda h: h.tensor_reduce(out=bsum[:], in_=cq[:], axis=AX.X, op=ALU.add), R=[cq], W=[bsum])
        k.op(dve, lambda h: h.tensor_tensor(out=bsum[:], in0=bsum[:], in1=bias_q[:], op=ALU.mult), R=[bsum, bias_q], W=[bsum])
        k.barrier()

    with ExitStack() as es:
        psf = [k.ps(f"psf{i}", [P, 4, P], F32, es) for i in range(6)]
        psb_t = [k.ps(f"psb{i}", [P, 2, 4, P], BF16, es) for i in range(2)]
        psb = []
        for t in psb_t:
            psb.append((t, 0))
            psb.append((t, 1))
        rr = {"f": 0, "b": 0}

        def nf():
            rr["f"] += 1
            return psf[rr["f"] % 6]

        def nb():
            rr["b"] += 1
            t, s = psb[rr["b"] % 4]
            return t, s

        xt = [k.sb(f"xt{i}", [P, D], F32, es) for i in range(2)]
        d_x = [k.dsem(f"x{i}") for i in range(2)]
        ssx = [k.sb(f"ssx{i}", [P, 1], F32, es) for i in range(2)]
        sqs = k.sb("sqs", [P, D], F32, es)
        hb = [k.sb(f"hb{i}", [P, D], BF16, es) for i in range(2)]
        hT = [k.sb(f"hT{i}", [P, KC, 516], BF16, es) for i in range(2)]
        hlast = [k.sb(f"hlast{i}", [P, KC, 2], BF16, es) for i in range(3)]
        praw = [k.sb(f"praw{i}", [P, 516], F32, es) for i in range(2)]
        cva = [k.sb(f"cva{i}", [P, 512], F32, es) for i in range(1)]
        qkv = [k.sb(f"qkv{i}", [P, 24, 512], BF16, es) for i in range(2)]
        bdt = [k.sb(f"bdt{i}", [P, 16], F32, es) for i in range(4)]
        gt = [k.sb(f"gt{i}", [P, 40], F32, es) for i in range(4)]
        eG = [k.sb(f"eG{i}", [P, 32], F32, es) for i in range(4)]
        S = [k.sb(f"S{i}", [P, 4, P], F32, es) for i in range(2)]
        Sb = [k.sb(f"Sb{i}", [P, 4, P], BF16, es) for i in range(2)]
        o_sb = [k.sb(f"o_sb{i}", [P, NH, P], F32, es) for i in range(2)]
        d_o = [k.dsem(f"o{i}") for i in range(2)]

        SINGLE = {"sq", "Gm"}

        NSET = 2

        def dbl(name, shape, dt):
            if name in SINGLE:
                t = k.sb(f"{name}0", shape, dt, es)
                return [t] * NSET
            return [k.sb(f"{name}{i}", shape, dt, es) for i in range(NSET)]
        T3 = [P, 4, P]
        k_tm, v_tm, q_tm = dbl("k_tm", T3, BF16), dbl("v_tm", T3, BF16), dbl("q_tm", T3, BF16)
        sq = dbl("sq", T3, F32)
        ss = dbl("ss", [P, 8], F32)
        sc = dbl("sc", [P, 24], F32)
        khat, kb_, kbg, kdec, qhat, qd, vb = [dbl(n, T3, BF16) for n in ("khat", "kb_", "kbg", "kdec", "qhat", "qd", "vb")]
        khatT, kbT, qhatT, qdT = [dbl(n, T3, BF16) for n in ("khatT", "kbT", "qhatT", "qdT")]
        Gm = dbl("Gm", T3, F32)
        ED, EDT = dbl("ED", T3, F32), dbl("EDT", T3, F32)
        EDTs = dbl("EDTs", T3, F32)
        EDm, EDTi = ED, EDT
        nA, nTA = qhat, qd
        nB, nTB = k_tm, q_tm
        AT = dbl("AT", T3, BF16)
        PTa, PTb = dbl("PTa", T3, BF16), v_tm
        u_sb = ED
        wT = khat
        vnb = kb_

        def rsqrt_(tl, ap, eps, mul=1.0):
            k.op(dve, lambda h: h.tensor_scalar(out=ap, in0=ap, scalar1=float(mul), scalar2=float(eps), op0=ALU.mult, op1=ALU.add), R=[tl], W=[tl])
            k.op(act, lambda h: h.activation(out=ap, in_=ap, func=AF.Ln), R=[tl], W=[tl])
            k.op(act, lambda h: h.activation(out=ap, in_=ap, func=AF.Exp, scale=-0.5), R=[tl], W=[tl])

        def bc_h(ap):
            return ap.unsqueeze(2).to_broadcast([P, 4, P])

        def bc_m(ap):
            return ap.unsqueeze(1).to_broadcast([P, 4, P])

        uid = [0]

        for dr in range(2 if stop >= 2 else 1):
            xsrc = xs[dr]
            for i in range(2):
                k.op(pool, lambda h: h.memset(S[i][:], 0.0), W=[S[i]])
                k.op(pool, lambda h: h.memset(Sb[i][:], 0.0), W=[Sb[i]])

            def stage_x(g):
                H = hT[g % 2]
                for ti in range(4):
                    t = g * 4 + ti
                    b = t % 2
                    k.dma(sp, lambda h: h.dma_start(out=xt[b][:], in_=xsrc[t * P:(t + 1) * P, :]), d_x[b], W=[xt[b]])
                    k.op(act, lambda h: h.activation(out=sqs[:], in_=xt[b][:], func=AF.Square), R=[xt[b]], W=[sqs])
                    k.op(dve, lambda h: h.tensor_reduce(out=ssx[b][:], in_=sqs[:], axis=AX.X, op=ALU.add), R=[sqs], W=[ssx[b]])
                    rsqrt_(ssx[b], ssx[b][:], EPS, 1.0 / D)
                    k.op(act, lambda h: h.activation(out=hb[b][:], in_=xt[b][:], func=AF.Copy, scale=ssx[b][:, 0:1]), R=[xt[b], ssx[b]], W=[hb[b]])
                    if debug and dr == 0 and t == 0:
                        k.dma(sp, lambda h: h.dma_start(out=dbg["ssx"][:], in_=ssx[b][:]), None, R=[ssx[b]], W=[dbg["ssx"]])
                        k.dma(sp, lambda h: h.dma_start(out=dbg["hb"][:], in_=hb[b][:]), None, R=[hb[b]], W=[dbg["hb"]])
                        k.dma(sp, lambda h: h.dma_start(out=dbg["xt"][:], in_=xt[b][:]), None, R=[xt[b]], W=[dbg["xt"]])
                        k.dma(sp, lambda h: h.dma_start(out=dbg["sqs"][:], in_=sqs[:]), None, R=[sqs], W=[dbg["sqs"]])
                    for half in range(2):
                        pt, s = nb()
                        for j in range(4):
                            kc = half * 4 + j
                            k.op(pe, lambda h: h.transpose(out=pt[:, s, j, :], in_=hb[b][:, kc * P:(kc + 1) * P], identity=ident_b),
                                 R=[hb[b], cstb], W=[pt])
                        k.op(act if half else dve, CP(act if half else dve, H[:, half * 4:half * 4 + 4, 2 + ti * P:2 + (ti + 1) * P], pt[:, s, :, :]), R=[pt], W=[H])
                k.op(pool, lambda h: h.tensor_copy(out=hlast[g % 3][:], in_=H[:, :, 512:514]), R=[H], W=[hlast[g % 3]])

            def stage_proj(g):
                H = hT[g % 2]
                Q = qkv[g % 2]
                if g > 0:
                    Hp = hlast[(g - 1) % 3]
                    k.op(pool, lambda h: h.tensor_copy(out=H[:, :, 0:2], in_=Hp[:]), R=[Hp], W=[H])
                else:
                    k.op(pool, lambda h: h.memset(H[:, :, 0:2], 0.0), W=[H])
                if g < NG - 1:
                    Hn = hT[(g + 1) % 2]
                    k.op(pool, lambda h: h.tensor_copy(out=H[:, :, 514:516], in_=Hn[:, :, 2:4]), R=[Hn], W=[H])
                else:
                    k.op(pool, lambda h: h.memset(H[:, :, 514:516], 0.0), W=[H])
                for cc in range(24):
                    pm = nf()
                    pmf = pm[:].rearrange("p a b -> p (a b)")
                    ph = nf()
                    phf = ph[:].rearrange("p a b -> p (a b)")
                    for kc in range(KC):
                        k.op(pe, lambda h: h.matmul(pmf, lhsT=wq[:, kc, cc * P:(cc + 1) * P], rhs=H[:, kc, 0:512],
                                                    start=(kc == 0), stop=(kc == KC - 1)), R=[wq, H], W=[pm])
                    for kc in range(KC):
                        k.op(pe, lambda h: h.matmul(phf[:, 0:4], lhsT=wq[:, kc, cc * P:(cc + 1) * P], rhs=H[:, kc, 512:516],
                                                    start=(kc == 0), stop=(kc == KC - 1)), R=[wq, H], W=[ph])
                    pr = praw[cc % 2]
                    k.op(act, lambda h: h.activation(out=pr[:, 0:512], in_=pmf, func=AF.Copy), R=[pm], W=[pr])
                    k.op(act, lambda h: h.activation(out=pr[:, 512:516], in_=phf[:, 0:4], func=AF.Copy), R=[ph], W=[pr])
                    ca = cva[0]
                    tp = (lambda t_: cq[:, cc, t_:t_ + 1]) if dr == 0 else (lambda t_: cq[:, cc, 4 - t_:5 - t_])
                    k.op(dve, lambda h: h.tensor_scalar(out=ca[:], in0=pr[:, 0:512], scalar1=tp(0), scalar2=None, op0=ALU.mult), R=[pr, cq], W=[ca])
                    k.op(dve, lambda h: h.scalar_tensor_tensor(out=ca[:], in0=pr[:, 1:513], scalar=tp(1), in1=ca[:], op0=ALU.mult, op1=ALU.add),
                         R=[pr, cq, ca], W=[ca])
                    k.op(dve, lambda h: h.scalar_tensor_tensor(out=ca[:], in0=pr[:, 2:514], scalar=tp(2), in1=ca[:], op0=ALU.mult, op1=ALU.add),
                         R=[pr, cq, ca], W=[ca])
                    k.op(dve, lambda h: h.scalar_tensor_tensor(out=ca[:], in0=pr[:, 3:515], scalar=tp(3), in1=ca[:], op0=ALU.mult, op1=ALU.add),
                         R=[pr, cq, ca], W=[ca])
                    edge = (g == 0 or g == NG - 1)
                    if edge:
                        k.op(dve, lambda h: h.scalar_tensor_tensor(out=ca[:], in0=pr[:, 4:516], scalar=tp(4), in1=ca[:], op0=ALU.mult, op1=ALU.add),
                             R=[pr, cq, ca], W=[ca])
                    else:
                        k.op(dve, lambda h: h.scalar_tensor_tensor(out=Q[:, cc, :], in0=pr[:, 4:516], scalar=tp(4), in1=ca[:], op0=ALU.mult, op1=ALU.add),
                             R=[pr, cq, ca], W=[Q])
                    if g == 0:
                        for t_ in range(2):
                            for m in range(2 - t_):
                                k.op(dve, lambda h: h.scalar_tensor_tensor(out=ca[:, t_:t_ + 1], in0=bias_q[:, cc:cc + 1], scalar=tp(m), in1=ca[:, t_:t_ + 1],
                                                                           op0=ALU.mult, op1=ALU.subtract), R=[bias_q, cq, ca], W=[ca])
                                k.op(dve, lambda h: h.tensor_scalar(out=ca[:, t_:t_ + 1], in0=ca[:, t_:t_ + 1], scalar1=-1.0, scalar2=None, op0=ALU.mult),
                                     R=[ca], W=[ca])
                    if g == NG - 1:
                        for t_ in range(2):
                            col = 511 - t_
                            for m in range(2 - t_):
                                k.op(dve, lambda h: h.scalar_tensor_tensor(out=ca[:, col:col + 1], in0=bias_q[:, cc:cc + 1], scalar=tp(4 - m),
                                                                           in1=ca[:, col:col + 1], op0=ALU.mult, op1=ALU.subtract), R=[bias_q, cq, ca], W=[ca])
                                k.op(dve, lambda h: h.tensor_scalar(out=ca[:, col:col + 1], in0=ca[:, col:col + 1], scalar1=-1.0, scalar2=None, op0=ALU.mult),
                                     R=[ca], W=[ca])
                    if edge:
                        k.op(dve, lambda h: h.tensor_copy(out=Q[:, cc, :], in_=ca[:]), R=[ca], W=[Q])
                    yield
                for cc in range(24):
                    k.op(act, lambda h: h.activation(out=Q[:, cc, :], in_=Q[:, cc, :], func=AF.Silu, bias=bsum[:, cc:cc + 1]), R=[Q, bsum], W=[Q])
                yield

            def stage_gates(g, ti):
                H = hT[g % 2]
                t = g * 4 + ti
                b = ti
                pm = nf()
                pmf = pm[:].rearrange("p a b -> p (a b)")
                for kc in range(KC):
                    k.op(pe, lambda h: h.matmul(pmf[:, 0:16], lhsT=H[:, kc, 2 + ti * P:2 + (ti + 1) * P], rhs=wbd[:, kc, dr * 16:(dr + 1) * 16],
                                                start=(kc == 0), stop=(kc == KC - 1)), R=[H, wbd], W=[pm])
                B, Gt, Eg = bdt[b], gt[b], eG[b]
                k.op(dve, lambda h: h.tensor_tensor(out=B[:], in0=pmf[:, 0:16], in1=bias_bd[:, dr * 16:(dr + 1) * 16], op=ALU.add),
                     R=[pm, bias_bd], W=[B])
                k.op(act, lambda h: h.activation(out=Gt[:, 0:8], in_=B[:, 0:8], func=AF.Exp, scale=-1.0), R=[B], W=[Gt])
                k.op(dve, lambda h: h.tensor_scalar(out=Gt[:, 0:8], in0=Gt[:, 0:8], scalar1=1.0, scalar2=None, op0=ALU.add), R=[Gt], W=[Gt])
                k.op(dve, lambda h: h.reciprocal(out=Gt[:, 8:16], in_=Gt[:, 0:8]), R=[Gt], W=[Gt])
                k.op(dve, lambda h: h.tensor_tensor(out=Gt[:, 16:24], in0=B[:, 8:16], in1=gpar[:, 16 + dr * 8:24 + dr * 8], op=ALU.add),
                     R=[B, gpar], W=[Gt])
                k.op(act, lambda h: h.activation(out=Gt[:, 16:24], in_=Gt[:, 16:24], func=AF.Exp), R=[Gt], W=[Gt])
                k.op(act, lambda h: h.activation(out=Gt[:, 16:24], in_=Gt[:, 16:24], func=AF.Ln, bias=1.0), R=[Gt], W=[Gt])
                k.op(dve, lambda h: h.tensor_tensor(out=Gt[:, 24:32], in0=Gt[:, 16:24], in1=gpar[:, dr * 8:dr * 8 + 8], op=ALU.mult),
                     R=[Gt, gpar], W=[Gt])
                pg = nf()
                pgf = pg[:].rearrange("p a b -> p (a b)")
                for i, m in enumerate((U_f, SUx_f, OC0_f, OC1_f)):
                    k.op(pe, lambda h: h.matmul(pgf[:, i * 8:(i + 1) * 8], lhsT=m, rhs=Gt[:, 24:32], start=True, stop=True), R=[cst, Gt], W=[pg])
                k.op(act, lambda h: h.activation(out=Eg[:], in_=pgf[:, 0:32], func=AF.Exp), R=[pg], W=[Eg])
                return Gt, Eg

            def unit(g, ti, hg, Gt, Eg):
                uid[0] += 1
                z = uid[0] % NSET
                Q = qkv[g % 2]
                cs = slice(ti * P, (ti + 1) * P)
                hs = slice(4 * hg, 4 * hg + 4)
                beta = Gt[:, 8 + 4 * hg:12 + 4 * hg]
                gg = Gt[:, 24 + 4 * hg:28 + 4 * hg]
                eGc = Eg[:, 4 * hg:4 * hg + 4]
                eR = Eg[:, 8 + 4 * hg:12 + 4 * hg]
                for base, dst, eng in ((8, k_tm[z], act), (16, v_tm[z], act), (0, q_tm[z], act)):
                    pt, s = nb()
                    for j in range(4):
                        k.op(pe, lambda h: h.transpose(out=pt[:, s, j, :], in_=Q[:, base + 4 * hg + j, cs], identity=ident_b), R=[Q, cstb], W=[pt])
                    k.op(eng, CP(eng, dst[:], pt[:, s, :, :]), R=[pt], W=[dst])
                    yield
                SS, SC = ss[z], sc[z]
                k.op(act, lambda h: h.activation(out=sq[z][:], in_=k_tm[z][:], func=AF.Square), R=[k_tm[z]], W=[sq[z]])
                k.op(dve, lambda h: h.tensor_reduce(out=SS[:, 0:4], in_=sq[z][:], axis=AX.X, op=ALU.add), R=[sq[z]], W=[SS])
                k.op(act, lambda h: h.activation(out=sq[z][:], in_=q_tm[z][:], func=AF.Square), R=[q_tm[z]], W=[sq[z]])
                k.op(dve, lambda h: h.tensor_reduce(out=SS[:, 4:8], in_=sq[z][:], axis=AX.X, op=ALU.add), R=[sq[z]], W=[SS])
                rsqrt_(SS, SS[:], EPS)
                yield
                rk, rq = SS[:, 0:4], SS[:, 4:8]
                tt = lambda o, a, b_: k.op(dve, lambda h: h.tensor_tensor(out=o, in0=a, in1=b_, op=ALU.mult), R=[SS, SC, Gt, Eg], W=[SC])
                s_kb, s_kbg, s_kd, s_q, s_qd = [SC[:, 4 * i:4 * i + 4] for i in range(5)]
                tt(s_kb, rk, beta)
                tt(s_kbg, s_kb, eGc)
                tt(s_kd, rk, eR)
                k.op(dve, lambda h: h.tensor_scalar(out=s_q, in0=rq, scalar1=float(P) ** -0.5, scalar2=None, op0=ALU.mult), R=[SS], W=[SC])
                tt(s_qd, s_q, eGc)
                def scl(eng, dst, src, s_ap, extra):
                    if eng is pool:
                        eng = dve
                    k.op(eng, lambda h: h.tensor_tensor(out=dst[:], in0=src[:], in1=bc_h(s_ap), op=ALU.mult), R=[src] + extra, W=[dst])
                scl(dve, khat[z], k_tm[z], rk, [SS])
                scl(pool, kb_[z], k_tm[z], s_kb, [SC])
                scl(dve, kbg[z], k_tm[z], s_kbg, [SC])
                scl(pool, kdec[z], k_tm[z], s_kd, [SC])
                scl(dve, qhat[z], q_tm[z], s_q, [SC])
                scl(pool, qd[z], q_tm[z], s_qd, [SC])
                scl(pool, vb[z], v_tm[z], beta, [Gt])
                yield
                for src, dst, eng in ((khat[z], khatT[z], act), (kb_[z], kbT[z], act), (qhat[z], qhatT[z], act), (qd[z], qdT[z], act)):
                    pt, s = nb()
                    for j in range(4):
                        k.op(pe, lambda h: h.transpose(out=pt[:, s, j, :], in_=src[:, j, :], identity=ident_b), R=[src, cstb], W=[pt])
                    k.op(eng, CP(eng, dst[:], pt[:, s, :, :]), R=[pt], W=[dst])
                    yield
                k.op(dve, lambda h: h.tensor_tensor(out=Gm[z][:], in0=bc_m(SUx_f), in1=bc_h(gg), op=ALU.mult), R=[cst, Gt], W=[Gm[z]])
                pD, pDT = nf(), nf()
                for j in range(4):
                    k.op(pe, lambda h: h.matmul(pD[:, j, :], lhsT=U_f, rhs=Gm[z][:, j, :], start=True, stop=True), R=[cst, Gm[z]], W=[pD])
                for j in range(4):
                    k.op(pe, lambda h: h.matmul(pDT[:, j, :], lhsT=Gm[z][:, j, :], rhs=U_f, start=True, stop=True), R=[cst, Gm[z]], W=[pDT])
                k.op(act, lambda h: h.activation(out=ED[z][:], in_=pD[:], func=AF.Exp), R=[pD], W=[ED[z]])
                k.op(act, lambda h: h.activation(out=EDT[z][:], in_=pDT[:], func=AF.Exp), R=[pDT], W=[EDT[z]])
                yield
                k.op(dve, lambda h: h.tensor_tensor(out=EDm[z][:], in0=ED[z][:], in1=bc_m(SLneg_f), op=ALU.mult), R=[ED[z], cst], W=[EDm[z]])
                k.op(dve, lambda h: h.tensor_tensor(out=EDTs[z][:], in0=EDT[z][:], in1=bc_m(SUneg_f), op=ALU.mult), R=[EDT[z], cst], W=[EDTs[z]])
                k.op(dve, lambda h: h.tensor_tensor(out=EDTi[z][:], in0=EDT[z][:], in1=bc_m(SUinc_f), op=ALU.mult), R=[EDT[z], cst], W=[EDTi[z]])
                yield
                pN, pNT, pA = nf(), nf(), nf()
                for j in range(4):
                    k.op(pe, lambda h: h.matmul(pN[:, j, :], lhsT=kbT[z][:, j, :], rhs=khatT[z][:, j, :], start=True, stop=True),
                         R=[kbT[z], khatT[z]], W=[pN])
                for j in range(4):
                    k.op(pe, lambda h: h.matmul(pNT[:, j, :], lhsT=khatT[z][:, j, :], rhs=kbT[z][:, j, :], start=True, stop=True),
                         R=[kbT[z], khatT[z]], W=[pNT])
                for j in range(4):
                    k.op(pe, lambda h: h.matmul(pA[:, j, :], lhsT=khatT[z][:, j, :], rhs=qhatT[z][:, j, :], start=True, stop=True),
                         R=[qhatT[z], khatT[z]], W=[pA])
                k.op(dve, lambda h: h.tensor_tensor(out=nA[z][:], in0=pN[:], in1=EDm[z][:], op=ALU.mult), R=[pN, EDm[z]], W=[nA[z]])
                k.op(dve, lambda h: h.tensor_tensor(out=nTA[z][:], in0=pNT[:], in1=EDTs[z][:], op=ALU.mult), R=[pNT, EDTs[z]], W=[nTA[z]])
                k.op(dve, lambda h: h.tensor_tensor(out=AT[z][:], in0=pA[:], in1=EDTi[z][:], op=ALU.mult), R=[pA, EDTi[z]], W=[AT[z]])
                yield
                k.op(dve, lambda h: h.tensor_tensor(out=PTa[z][:], in0=nTA[z][:], in1=bc_m(ident_f), op=ALU.add), R=[nTA[z], cst], W=[PTa[z]])
                n_c, nT_c, n_n, nT_n = nA[z], nTA[z], nB[z], nTB[z]
                PT_c, PT_n = PTa[z], PTb[z]
                for it in range(5):
                    p1 = nf()
                    for j in range(4):
                        k.op(pe, lambda h: h.matmul(p1[:, j, :], lhsT=nT_c[:, j, :], rhs=n_c[:, j, :], start=True, stop=True), R=[nT_c, n_c], W=[p1])
                    if it < 4:
                        p2 = nf()
                        for j in range(4):
                            k.op(pe, lambda h: h.matmul(p2[:, j, :], lhsT=n_c[:, j, :], rhs=nT_c[:, j, :], start=True, stop=True), R=[nT_c, n_c], W=[p2])
                    k.op(act, lambda h: h.activation(out=n_n[:], in_=p1[:], func=AF.Copy), R=[p1], W=[n_n])
                    if it < 4:
                        k.op(act, lambda h: h.activation(out=nT_n[:], in_=p2[:], func=AF.Copy), R=[p2], W=[nT_n])
                    yield
                    p3 = nf()
                    for j in range(4):
                        k.op(pe, lambda h: h.matmul(p3[:, j, :], lhsT=n_n[:, j, :], rhs=PT_c[:, j, :], start=True, stop=True), R=[n_n, PT_c], W=[p3])
                    k.op(dve, lambda h: h.tensor_tensor(out=PT_n[:], in0=p3[:], in1=PT_c[:], op=ALU.add), R=[p3, PT_c], W=[PT_n])
                    yield
                    n_c, n_n = n_n, n_c
                    nT_c, nT_n = nT_n, nT_c
                    PT_c, PT_n = PT_n, PT_c
                PT = PT_c
                pU, pW = nf(), nf()
                for j in range(4):
                    k.op(pe, lambda h: h.matmul(pU[:, j, :], lhsT=PT[:, j, :], rhs=vb[z][:, j, :], start=True, stop=True), R=[PT, vb[z]], W=[pU])
                for j in range(4):
                    k.op(pe, lambda h: h.matmul(pW[:, j, :], lhsT=kbg[z][:, j, :], rhs=PT[:, j, :], start=True, stop=True), R=[PT, kbg[z]], W=[pW])
                k.op(act, lambda h: h.activation(out=u_sb[z][:], in_=pU[:], func=AF.Copy), R=[pU], W=[u_sb[z]])
                k.op(act, lambda h: h.activation(out=wT[z][:], in_=pW[:], func=AF.Copy), R=[pW], W=[wT[z]])
                yield
                while prog[hg] != g * 4 + ti:
                    yield
                O = o_sb[(g * 4 + ti) % 2]
                St, Sbt = S[hg], Sb[hg]
                for c in range(2):
                    rs = slice(64 * c, 64 * c + 64)
                    p1 = nf()
                    for j in range(4):
                        k.op(pe, lambda h: h.matmul(p1[:, j, :], lhsT=wT[z][:, j, :], rhs=Sbt[:, j, :], start=True, stop=True), R=[wT[z], Sbt], W=[p1])
                    k.op(dve, lambda h: h.tensor_tensor(out=vnb[z][rs, :, :], in0=u_sb[z][rs, :, :], in1=p1[rs, :, :], op=ALU.subtract),
                         R=[u_sb[z], p1], W=[vnb[z]])
                    yield
                    p2, p3 = nf(), nf()
                    for j in range(4):
                        k.op(pe, lambda h: h.matmul(p2[:, j, :], lhsT=qdT[z][:, j, :], rhs=Sbt[:, j, :], start=True, stop=False), R=[qdT[z], Sbt], W=[p2])
                        k.op(pe, lambda h: h.matmul(p2[:, j, :], lhsT=AT[z][rs, j, :], rhs=vnb[z][rs, j, :], start=False, stop=True), R=[AT[z], vnb[z]], W=[p2])
                    for j in range(4):
                        k.op(pe, lambda h: h.matmul(p3[:, j, :], lhsT=kdec[z][rs, j, :], rhs=vnb[z][rs, j, :], start=True, stop=True), R=[kdec[z], vnb[z]], W=[p3])
                    k.op(act, lambda h: h.activation(out=O[rs, hs, :], in_=p2[rs, :, :], func=AF.Copy), R=[p2], W=[O])
                    egt = Eg[:, 16 + 8 * c + 4 * hg:20 + 8 * c + 4 * hg]
                    k.op(dve, lambda h: h.tensor_tensor(out=St[:], in0=St[:], in1=bc_h(egt), op=ALU.mult), R=[St, Eg], W=[St])
                    k.op(dve, lambda h: h.tensor_tensor(out=St[:], in0=St[:], in1=p3[:], op=ALU.add), R=[St, p3], W=[St])
                    k.op(act, lambda h: h.activation(out=Sbt[:], in_=St[:], func=AF.Copy), R=[St], W=[Sbt])
                    yield
                prog[hg] += 1

            prog = {0: 0, 1: 0}
            stage_x(0)
            if NG > 1:
                stage_x(1)
            for _ in stage_proj(0):
                pass
            WIN = NSET
            for g in range(NG):
                gates = [stage_gates(g, ti) for ti in range(4)]
                if g + 2 < NG:
                    stage_x(g + 2)
                side = [stage_proj(g + 1)] if g + 1 < NG else []
                if debug and dr == 0 and g == 0:
                    Gt, Eg = gates[0]
                    k.dma(sp, lambda h: h.dma_start(out=dbg["gt"][:], in_=Gt[:]), None, R=[Gt], W=[dbg["gt"]])
                    k.dma(sp, lambda h: h.dma_start(out=dbg["eg"][:], in_=Eg[:]), None, R=[Eg], W=[dbg["eg"]])
                    k.dma(sp, lambda h: h.dma_start(out=dbg["qkv"][:], in_=qkv[0][:]), None, R=[qkv[0]], W=[dbg["qkv"]])
                todo = [(ti, hg) for ti in range(4) for hg in range(2)]
                active, done = [], {ti: 0 for ti in range(4)}
                while todo or active or side:
                    while len(active) < WIN and todo:
                        ti, hg = todo.pop(0)
                        active.append((ti, unit(g, ti, hg, gates[ti][0], gates[ti][1])))
                    for item in list(active):
                        ti, gen = item
                        try:
                            next(gen)
                        except StopIteration:
                            active.remove(item)
                            done[ti] += 1
                            if done[ti] == 2:
                                t = g * 4 + ti
                                O = o_sb[t % 2]
                                k.dma(sp, lambda h: h.dma_start(out=o_dram[dr][t * P:(t + 1) * P, :], in_=O[:].rearrange("p a b -> p (a b)")),
                                      d_o[t % 2], R=[O], W=[o_dram[dr]])
                    for sg_ in list(side):
                        try:
                            next(sg_)
                        except StopIteration:
                            side.remove(sg_)
        k.barrier()
    es1.close()

    if stop >= 3:
        phase2(nc, k, TSEQ, mod, s1, cst, cstb, o_dram, din, debug, stop)
    if stop < 5:
        name = "out" if stop < 3 else "out_probe"
        outp = Tl(nc.dram_tensor(name, [TSEQ // 4, D], F32, kind="ExternalOutput").ap())
        k.dma(sp, lambda h: h.dma_start(out=outp[0:P, 0:P], in_=cst[:, 0, :]), None, R=[cst], W=[outp])
    k.barrier()
    k.es.close()
    return nc


def phase2(nc, k, TSEQ, mod, s1, cst, cstb, o_dram, din, debug, stop=99):
    pe, act, dve, pool, sp = k.pe, k.act, k.dve, k.pool, k.sp
    TQ = TSEQ // 4
    NTQ = TQ // P
    GW = P
    NGQ = TQ // GW
    TPG = GW // P
    CAP = P * int(np.ceil(CAPF * TQ / 8 / P))
    CT = CAP // P
    NSLOT = NE * CAP
    BIG = float(NSLOT + 64)
    xo = din("xo", [TQ + 2, D])
    hmask = din("hmask", [P, 2])
    idx_in = din("idx", [P, 2, NTQ], I32)
    w_z, w_sc, w_g = din("w_z", [D, D]), din("w_sc", [D, 3 * D]), din("w_g", [D, 2 * D])
    convs_fm = din("convs_fm", [P, 8, 3])
    onorm = din("onorm", [1, P])
    w_up_a, w_out_sc, w_o = din("w_up_a", [D, D]), din("w_out_sc", [D, D]), din("w_o", [D, D])
    n2_fm = din("n2_fm", [P, KC])
    router_w = din("router_w", [D, NE])
    router_b = din("router_b", [1, NE])
    ecap = din("ecap", [1, NE])
    fnw = din("fnw", [1, D])
    moe_w1 = din("moe_w1", [NE, D, 2 * D])
    moe_b1_fm = din("moe_b1_fm", [P, NE, 16])
    moe_w2 = din("moe_w2", [NE, D, D])
    moe_b2 = din("moe_b2", [NE, D])
    out = Tl(nc.dram_tensor("out", [TQ, D], F32, kind="ExternalOutput").ap())
    x1_d = Tl(nc.dram_tensor("x1_d", [TQ, D], F32).ap())
    xbuf = Tl(nc.dram_tensor("xbuf", [NSLOT, D], BF16).ap())
    ybuf = Tl(nc.dram_tensor("ybuf", [NSLOT, D], F32).ap())
    ident_f, ident_b = cst[:, 0, :], cstb[:, 0, :]
    bc_o = nc.gpsimd.to_reg(TSEQ - 1)
    bc_s = nc.gpsimd.to_reg(NSLOT - 1)
    SUfull_b, ONES_b = cstb[:, 8, :], cstb[:, 9, :]

    def bc_h(ap, n, m):
        return ap.unsqueeze(2).to_broadcast([P, n, m])

    def rsqrt_(tl, ap, eps, mul=1.0):
        k.op(dve, lambda h: h.tensor_scalar(out=ap, in0=ap, scalar1=float(mul), scalar2=float(eps), op0=ALU.mult, op1=ALU.add), R=[tl], W=[tl])
        k.op(act, lambda h: h.activation(out=ap, in_=ap, func=AF.Ln), R=[tl], W=[tl])
        k.op(act, lambda h: h.activation(out=ap, in_=ap, func=AF.Exp, scale=-0.5), R=[tl], W=[tl])

    vbc = k.sb("vbc", [P, 4, D], F32)
    dki = k.sb("dki", [P, NTQ, 4], I32)
    gk = k.sb("gk", [P, NTQ, 4], F32)
    d_m = k.dsem("m")

    with ExitStack() as es:
        W2 = k.sb("W2", [P, KC, 6 * D], BF16, es)
        Wp = [k.sb(f"Wp{i}", [P, KC, D], BF16, es) for i in range(3)]
        bias2 = k.sb("bias2", [P, 48], F32, es)
        bz_bc = k.sb("bz_bc", [P, D], F32, es)
        cs3 = k.sb("cs3", [P, 8, 3], F32, es)
        on_bc = k.sb("on_bc", [P, P], F32, es)
        rb_bc = k.sb("rb_bc", [P, NE], F32, es)
        ec_bc = k.sb("ec_bc", [P, NE], F32, es)
        hm = k.sb("hm", [P, 2], F32, es)
        n2 = k.sb("n2", [P, KC], F32, es)
        s2 = k.sb("s2", [P, KC], F32, es)
        rw = k.sb("rw", [P, KC, NE], F32, es)
        idx = k.sb("idx_sb", [P, 2, NTQ], I32, es)
        selb = k.sb("selb", [P, NTQ, NE], BF16, es)
        psf = [k.ps(f"p2f{i}", [P, 512], F32, es) for i in range(6)]
        psb_t = [k.ps(f"p2b{i}", [P, 2, 4, P], BF16, es) for i in range(2)]
        rr = {"f": 0, "b": 0}

        def nf():
            rr["f"] += 1
            return psf[rr["f"] % 6]

        def nb():
            rr["b"] += 1
            return psb_t[(rr["b"] // 2) % 2], rr["b"] % 2

        for dst, src in ((cs3, convs_fm), (hm, hmask), (n2, n2_fm), (idx, idx_in)):
            k.dma(sp, lambda h: h.dma_start(out=dst[:], in_=src), None, W=[dst])
        k.dma(sp, lambda h: h.dma_start(out=on_bc[:], in_=onorm.partition_broadcast(P)), None, W=[on_bc])
        k.dma(sp, lambda h: h.dma_start(out=rb_bc[:], in_=router_b.partition_broadcast(P)), None, W=[rb_bc])
        k.dma(sp, lambda h: h.dma_start(out=ec_bc[:], in_=ecap.partition_broadcast(P)), None, W=[ec_bc])
        k.dma(sp, lambda h: h.dma_start(out=rw[:], in_=router_w.rearrange("(k p) e -> p k e", p=P)), None, W=[rw])
        for i, src in enumerate((w_up_a, w_out_sc, w_o)):
            k.dma(pool, lambda h: h.dma_start(out=Wp[i][:], in_=src.rearrange("(k p) c -> p k c", p=P)), None, W=[Wp[i]])
        k.op(dve, lambda h: h.scalar_tensor_tensor(out=s2[:], in0=mod[:, 32:40], scalar=1.0, in1=n2[:], op0=ALU.add, op1=ALU.mult), R=[mod, n2], W=[s2])
        with ExitStack() as esw:
            BW = 256
            wst = [k.sb(f"w2st{i}", [P, KC, BW], F32, esw) for i in range(2)]
            d_w = [k.dsem(f"w2{i}") for i in range(2)]
            shb = k.sb("shb2", [P, KC, P], F32, esw)
            vb_l = k.sb("vb_l", [P, P], F32, esw)
            pb = psf[5]
            pz = [psf[3], psf[4]]
            for kc in range(KC):
                k.op(pool, lambda h: h.tensor_copy(out=shb[:, kc, :], in_=mod[:, kc:kc + 1].to_broadcast([P, P])), R=[mod], W=[shb])
            srcs = [(w_z, c0) for c0 in range(0, D, BW)] + [(w_sc, c0) for c0 in range(0, 3 * D, BW)] + [(w_g, c0) for c0 in range(0, 2 * D, BW)]
            for blk, (src, c0) in enumerate(srcs):
                w = wst[blk % 2]
                k.dma(sp, lambda h: h.dma_start(out=w[:], in_=src[:, c0:c0 + BW].rearrange("(k p) c -> p k c", p=P)), d_w[blk % 2], W=[w])
                for j in range(BW // P):
                    cc = blk * (BW // P) + j
                    for kc in range(KC):
                        k.op(pe, lambda h: h.matmul(pb[:, cc:cc + 1], lhsT=w[:, kc, j * P:(j + 1) * P], rhs=mod[:, kc:kc + 1],
                                                    start=(kc == 0), stop=(kc == KC - 1)), R=[w, mod], W=[pb])
                if blk < D // BW:
                    pzt = pz[blk % 2]
                    for kc in range(KC):
                        k.op(pe, lambda h: h.matmul(pzt[:, 0:BW], lhsT=shb[:, kc, :], rhs=w[:, kc, :], start=(kc == 0), stop=(kc == KC - 1)),
                             R=[shb, w], W=[pzt])
                    k.op(act, CP(act, bz_bc[:, blk * BW:(blk + 1) * BW], pzt[:, 0:BW]), R=[pzt], W=[bz_bc])
                k.op(dve if blk % 2 else pool, lambda h: h.tensor_tensor(out=W2[:, :, blk * BW:(blk + 1) * BW], in0=w[:], in1=bc_h(s1[:, :], KC, BW), op=ALU.mult),
                     R=[w, s1], W=[W2])
            k.op(dve, lambda h: h.tensor_copy(out=bias2[:], in_=pb[:, 0:48]), R=[pb], W=[bias2])
            for vi, (vt, c0) in enumerate(((mod, 16), (s2, 0), (mod, 24), (mod, 40))):
                for half in range(2):
                    pv = nf()
                    for j in range(4):
                        kc = half * 4 + j
                        k.op(pool, lambda h: h.tensor_copy(out=vb_l[:], in_=vt[:, c0 + kc:c0 + kc + 1].to_broadcast([P, P])), R=[vt], W=[vb_l])
                        k.op(pe, lambda h: h.matmul(pv[:, j * P:(j + 1) * P], lhsT=vb_l[:], rhs=ident_f, start=True, stop=True), R=[vb_l, cst], W=[pv])
                    k.op(act, CP(act, vbc[:, vi, half * 512:(half + 1) * 512], pv[:]), R=[pv], W=[vbc])
            k.barrier()
        gtm_bc, s2_bc, shf_bc, gtf_bc = [vbc[:, i, :] for i in range(4)]

        xres = k.sb("xres", [P, TPG, D], F32, es)
        d_x = [k.dsem(f"x2{i}") for i in range(TPG)]
        d_xh = k.dsem("xh2")
        ssx = k.sb("ssx2", [P, 4], F32, es)
        hb = k.sb("hb2", [P, D], BF16, es)
        hT = k.sb("hT2", [P, KC, GW + 2], BF16, es)
        c_sb = k.sb("c_sb", [P, GW + 2], F32, es)
        cu = k.sb("cu", [P, GW + 2], F32, es)
        cv = k.sb("cv", [P, GW], F32, es)
        ybin = k.sb("ybin", [P, KC, GW], BF16, es)
        ogT = k.sb("ogT", [P, KC, GW], BF16, es)
        mrg = k.sb("mrg", [P, KC, GW], BF16, es)
        sga = k.sb("sga", [P, GW], F32, es)
        sgb = k.sb("sgb", [P, GW], F32, es)
        t1 = k.sb("t1", [P, GW], F32, es)
        t2 = k.sb("t2", [P, GW], F32, es)
        of_ = k.sb("of_", [P, NH, P], F32, es)
        x1v = of_[:].rearrange("p a b -> p (a b)")
        xhv = of_[0:2, :, :].rearrange("p a b -> p (a b)")
        d_g = k.dsem("g2")
        osq = k.sb("osq", [P, NH, P], F32, es)
        h2v = osq[:].rearrange("p a b -> p (a b)")
        oss = k.sb("oss", [P, NH], F32, es)
        zs = k.sb("zs", [P, D], F32, es)
        h2Tv = zs[:].rearrange("p (a b) -> p a b", a=KC)
        obv = zs[:].rearrange("p (a b) -> p a b", a=NH)
        d_s = k.dsem("s2")
        d_sc = k.dsem("sc2")
        d_zf = k.dsem("zf2")
        lg = k.sb("lg", [P, NE], F32, es)
        m8 = k.sb("m8", [P, 8], F32, es)
        rt = k.sb("rt", [P, 8, NE], F32, es)
        r1 = k.sb("r1c", [P, 8], F32, es)
        dkf = k.sb("dkf", [P, 4], F32, es)

        k.op(pool, lambda h: h.memset(hb[:], 0.0), W=[hb])
        xbv = xbuf[:, :].rearrange("(n p) d -> n p d", p=P)
        for n_ in range(NSLOT // P):
            k.dma(sp, lambda h: h.dma_start(out=xbv[n_], in_=hb[:]), d_zf, R=[hb], W=[xbuf])
        for g in range(NGQ):
            t0 = g * GW
            for ti in range(TPG):
                k.dma(sp, lambda h: h.dma_start(out=xres[:, ti, :], in_=xo[1 + t0 + ti * P:1 + t0 + (ti + 1) * P, :]), d_x[ti], W=[xres])
            k.dma(sp, lambda h: h.dma_start(out=xhv[0:1, :], in_=xo[t0:t0 + 1, :]), d_xh, W=[of_])
            k.dma(sp, lambda h: h.dma_start(out=xhv[1:2, :], in_=xo[t0 + GW + 1:t0 + GW + 2, :]), d_xh, W=[of_])
            for ti in range(TPG):
                k.op(act, lambda h: h.activation(out=zs[:], in_=xres[:, ti, :], func=AF.Square), R=[xres], W=[zs])
                k.op(dve, lambda h: h.tensor_reduce(out=ssx[:, 0:1], in_=zs[:], axis=AX.X, op=ALU.add), R=[zs], W=[ssx])
                rsqrt_(ssx, ssx[:, 0:1], EPS, 1.0 / D)
                k.op(act, lambda h: h.activation(out=hb[:], in_=xres[:, ti, :], func=AF.Copy, scale=ssx[:, 0:1]), R=[xres, ssx], W=[hb])
                for half in range(2):
                    pt, s = nb()
                    for j in range(4):
                        kc = half * 4 + j
                        k.op(pe, lambda h: h.transpose(out=pt[:, s, j, :], in_=hb[:, kc * P:(kc + 1) * P], identity=ident_b), R=[hb, cstb], W=[pt])
                    e_ = act if half else dve
                    k.op(e_, CP(e_, hT[:, half * 4:half * 4 + 4, 1 + ti * P:1 + (ti + 1) * P], pt[:, s, :, :]), R=[pt], W=[hT])
            k.op(act, lambda h: h.activation(out=zs[0:2, :], in_=xhv, func=AF.Square), R=[of_], W=[zs])
            k.op(dve, lambda h: h.tensor_reduce(out=ssx[0:2, 1:2], in_=zs[0:2, :], axis=AX.X, op=ALU.add), R=[zs], W=[ssx])
            rsqrt_(ssx, ssx[0:2, 1:2], EPS, 1.0 / D)
            k.op(act, lambda h: h.activation(out=hb[0:2, :], in_=xhv, func=AF.Copy, scale=ssx[0:2, 1:2]), R=[of_, ssx], W=[hb])
            for half in range(2):
                pt, s = nb()
                for j in range(4):
                    kc = half * 4 + j
                    k.op(pe, lambda h: h.transpose(out=pt[:, s, j, 0:2], in_=hb[0:2, kc * P:(kc + 1) * P], identity=cstb[0:2, 0, 0:2]), R=[hb, cstb], W=[pt])
                k.op(dve, lambda h: h.tensor_copy(out=hT[:, half * 4:half * 4 + 4, 0:1], in_=pt[:, s, :, 0:1]), R=[pt], W=[hT])
                k.op(dve, lambda h: h.tensor_copy(out=hT[:, half * 4:half * 4 + 4, GW + 1:GW + 2], in_=pt[:, s, :, 1:2]), R=[pt], W=[hT])

            def proj(col0, n0, n1):
                pm = nf()
                for kc in range(KC):
                    k.op(pe, lambda h: h.matmul(pm[:, 0:n1 - n0], lhsT=W2[:, kc, col0:col0 + P], rhs=hT[:, kc, n0:n1], start=(kc == 0), stop=(kc == KC - 1)),
                         R=[W2, hT], W=[pm])
                return pm

            for j in range(8):
                bc_, bu_, bb_ = bias2[:, 16 + j:17 + j], bias2[:, 24 + j:25 + j], bias2[:, 8 + j:9 + j]
                pc = proj(2 * D + j * P, 1, GW + 1)
                pch = proj(2 * D + j * P, 0, 1)
                pch2 = proj(2 * D + j * P, GW + 1, GW + 2)
                k.op(act, lambda h: h.activation(out=c_sb[:, 1:GW + 1], in_=pc[:, 0:GW], func=AF.Identity, bias=bc_), R=[pc, bias2], W=[c_sb])
                k.op(act, lambda h: h.activation(out=c_sb[:, 0:1], in_=pch[:, 0:1], func=AF.Identity, bias=bc_), R=[pch, bias2], W=[c_sb])
                k.op(act, lambda h: h.activation(out=c_sb[:, GW + 1:GW + 2], in_=pch2[:, 0:1], func=AF.Identity, bias=bc_), R=[pch2, bias2], W=[c_sb])
                pu = proj(3 * D + j * P, 1, GW + 1)
                puh = proj(3 * D + j * P, 0, 1)
                puh2 = proj(3 * D + j * P, GW + 1, GW + 2)
                k.op(dve, lambda h: h.scalar_tensor_tensor(out=cu[:, 1:GW + 1], in0=pu[:, 0:GW], scalar=bu_, in1=c_sb[:, 1:GW + 1], op0=ALU.add, op1=ALU.mult),
                     R=[pu, bias2, c_sb], W=[cu])
                k.op(dve, lambda h: h.scalar_tensor_tensor(out=cu[:, 0:1], in0=puh[:, 0:1], scalar=bu_, in1=c_sb[:, 0:1], op0=ALU.add, op1=ALU.mult),
                     R=[puh, bias2, c_sb], W=[cu])
                k.op(dve, lambda h: h.scalar_tensor_tensor(out=cu[:, GW + 1:GW + 2], in0=puh2[:, 0:1], scalar=bu_, in1=c_sb[:, GW + 1:GW + 2], op0=ALU.add, op1=ALU.mult),
                     R=[puh2, bias2, c_sb], W=[cu])
                if g == 0:
                    k.op(dve, lambda h: h.tensor_scalar(out=cu[:, 0:1], in0=cu[:, 0:1], scalar1=hm[:, 0:1], scalar2=None, op0=ALU.mult), R=[cu, hm], W=[cu])
                if g == NGQ - 1:
                    k.op(dve, lambda h: h.tensor_scalar(out=cu[:, GW + 1:GW + 2], in0=cu[:, GW + 1:GW + 2], scalar1=hm[:, 1:2], scalar2=None, op0=ALU.mult),
                         R=[cu, hm], W=[cu])
                k.op(dve, lambda h: h.tensor_scalar(out=cv[:], in0=cu[:, 0:GW], scalar1=cs3[:, j, 0:1], scalar2=None, op0=ALU.mult), R=[cu, cs3], W=[cv])
                k.op(dve, lambda h: h.scalar_tensor_tensor(out=cv[:], in0=cu[:, 1:GW + 1], scalar=cs3[:, j, 1:2], in1=cv[:], op0=ALU.mult, op1=ALU.add),
                     R=[cu, cs3, cv], W=[cv])
                k.op(dve, lambda h: h.scalar_tensor_tensor(out=cv[:], in0=cu[:, 2:GW + 2], scalar=cs3[:, j, 2:3], in1=cv[:], op0=ALU.mult, op1=ALU.add),
                     R=[cu, cs3, cv], W=[cv])
                pbm = proj(D + j * P, 1, GW + 1)
                k.op(dve, lambda h: h.scalar_tensor_tensor(out=ybin[:, j, :], in0=pbm[:, 0:GW], scalar=bb_, in1=cv[:], op0=ALU.add, op1=ALU.mult),
                     R=[pbm, bias2, cv], W=[ybin])

            for ti in range(TPG):
                it = g * TPG + ti
                k.dma(pool, lambda h: h.indirect_dma_start(out=of_[:].rearrange("p a b -> p (a b)"), out_offset=None, in_=o_dram[0][:, :],
                                                           in_offset=bass.IndirectOffsetOnAxis(ap=idx[:, 0, it:it + 1], axis=0),
                                                           bounds_check=bc_o, oob_is_err=False), d_g, R=[o_dram[0], idx], W=[of_])
                k.dma(pool, lambda h: h.indirect_dma_start(out=zs[:], out_offset=None, in_=o_dram[1][:, :],
                                                           in_offset=bass.IndirectOffsetOnAxis(ap=idx[:, 1, it:it + 1], axis=0),
                                                           bounds_check=bc_o, oob_is_err=False), d_g, R=[o_dram[1], idx], W=[zs])
                k.op(pool, lambda h: h.tensor_tensor(out=of_[:], in0=of_[:], in1=obv, op=ALU.add), R=[of_, zs], W=[of_])
                k.op(pool, lambda h: h.tensor_tensor(out=osq[:], in0=of_[:], in1=of_[:], op=ALU.mult), R=[of_], W=[osq])
                k.op(dve, lambda h: h.tensor_reduce(out=oss[:], in_=osq[:], axis=AX.X, op=ALU.add), R=[osq], W=[oss])
                rsqrt_(oss, oss[:], P * EPS)
                k.op(dve, lambda h: h.tensor_tensor(out=osq[:], in0=of_[:], in1=bc_h(oss[:, :], NH, P), op=ALU.mult), R=[of_, oss], W=[osq])
                k.op(pool, lambda h: h.tensor_tensor(out=osq[:], in0=osq[:], in1=on_bc[:].unsqueeze(1).to_broadcast([P, NH, P]), op=ALU.mult), R=[osq, on_bc], W=[osq])
                for half in range(2):
                    pz_ = nf()
                    for kc in range(KC):
                        k.op(pe, lambda h: h.matmul(pz_[:], lhsT=hT[:, kc, 1 + ti * P:1 + (ti + 1) * P], rhs=W2[:, kc, half * 512:(half + 1) * 512],
                                                    start=(kc == 0), stop=(kc == KC - 1)), R=[hT, W2], W=[pz_])
                    k.op(dve, lambda h: h.tensor_tensor(out=zs[:, half * 512:(half + 1) * 512], in0=pz_[:], in1=bz_bc[:, half * 512:(half + 1) * 512], op=ALU.add),
                         R=[pz_, bz_bc], W=[zs])
                k.op(act, lambda h: h.activation(out=zs[:], in_=zs[:], func=AF.Silu), R=[zs], W=[zs])
                k.op(dve, lambda h: h.scalar_tensor_tensor(out=hb[:], in0=osq[:].rearrange("p a b -> p (a b)"), scalar=float(P) ** 0.5, in1=zs[:], op0=ALU.mult, op1=ALU.mult),
                     R=[osq, zs], W=[hb])
                for half in range(2):
                    pt, s = nb()
                    for j in range(4):
                        kc = half * 4 + j
                        k.op(pe, lambda h: h.transpose(out=pt[:, s, j, :], in_=hb[:, kc * P:(kc + 1) * P], identity=ident_b), R=[hb, cstb], W=[pt])
                    e_ = act if half else dve
                    k.op(e_, CP(e_, ogT[:, half * 4:half * 4 + 4, ti * P:(ti + 1) * P], pt[:, s, :, :]), R=[pt], W=[ogT])

            for n in range(8):
                pga = proj(4 * D + n * P, 1, GW + 1)
                k.op(act, lambda h: h.activation(out=sga[:], in_=pga[:, 0:GW], func=AF.Sigmoid, bias=bias2[:, 32 + n:33 + n]), R=[pga, bias2], W=[sga])
                pgb = proj(5 * D + n * P, 1, GW + 1)
                k.op(act, lambda h: h.activation(out=sgb[:], in_=pgb[:, 0:GW], func=AF.Sigmoid, bias=bias2[:, 40 + n:41 + n]), R=[pgb, bias2], W=[sgb])
                pya, pyb = nf(), nf()
                for kc in range(KC):
                    k.op(pe, lambda h: h.matmul(pya[:, 0:GW], lhsT=Wp[0][:, kc, n * P:(n + 1) * P], rhs=ogT[:, kc, :], start=(kc == 0), stop=(kc == KC - 1)),
                         R=[Wp[0], ogT], W=[pya])
                for kc in range(KC):
                    k.op(pe, lambda h: h.matmul(pyb[:, 0:GW], lhsT=Wp[1][:, kc, n * P:(n + 1) * P], rhs=ybin[:, kc, :], start=(kc == 0), stop=(kc == KC - 1)),
                         R=[Wp[1], ybin], W=[pyb])
                k.op(dve, lambda h: h.tensor_tensor(out=t1[:], in0=pya[:, 0:GW], in1=sga[:], op=ALU.mult), R=[pya, sga], W=[t1])
                k.op(dve, lambda h: h.tensor_tensor(out=t2[:], in0=pyb[:, 0:GW], in1=sgb[:], op=ALU.mult), R=[pyb, sgb], W=[t2])
                k.op(pool, lambda h: h.tensor_tensor(out=mrg[:, n, :], in0=t1[:], in1=t2[:], op=ALU.add), R=[t1, t2], W=[mrg])

            for ti in range(TPG):
                it = g * TPG + ti
                for half in range(2):
                    pm = nf()
                    for kc in range(KC):
                        k.op(pe, lambda h: h.matmul(pm[:], lhsT=mrg[:, kc, ti * P:(ti + 1) * P], rhs=Wp[2][:, kc, half * 512:(half + 1) * 512],
                                                    start=(kc == 0), stop=(kc == KC - 1)), R=[mrg, Wp[2]], W=[pm])
                    hs_ = slice(half * 512, (half + 1) * 512)
                    k.op(dve, lambda h: h.tensor_tensor(out=x1v[:, hs_], in0=pm[:], in1=gtm_bc[:, hs_], op=ALU.mult), R=[pm, vbc], W=[of_])
                k.op(pool, lambda h: h.tensor_tensor(out=x1v, in0=x1v, in1=xres[:, ti, :], op=ALU.add), R=[of_, xres], W=[of_])
                k.dma(sp, lambda h: h.dma_start(out=x1_d[it * P:(it + 1) * P, :], in_=x1v), d_s, R=[of_], W=[x1_d])
                k.op(act, lambda h: h.activation(out=h2v, in_=x1v, func=AF.Square), R=[of_], W=[osq])
                k.op(dve, lambda h: h.tensor_reduce(out=ssx[:, 2:3], in_=h2v, axis=AX.X, op=ALU.add), R=[osq], W=[ssx])
                rsqrt_(ssx, ssx[:, 2:3], D * EPS)
                k.op(dve, lambda h: h.scalar_tensor_tensor(out=h2v, in0=x1v, scalar=ssx[:, 2:3], in1=s2_bc, op0=ALU.mult, op1=ALU.mult), R=[of_, ssx, vbc], W=[osq])
                k.op(dve, lambda h: h.scalar_tensor_tensor(out=h2v, in0=h2v, scalar=float(D) ** 0.5, in1=shf_bc, op0=ALU.mult, op1=ALU.add), R=[osq, vbc], W=[osq])
                k.op(act, CP(act, hb[:], h2v), R=[osq], W=[hb])
                for half in range(2):
                    pt = nf()
                    for j in range(4):
                        kc = half * 4 + j
                        k.op(pe, lambda h: h.transpose(out=pt[:, j * P:(j + 1) * P], in_=h2v[:, kc * P:(kc + 1) * P], identity=ident_f), R=[osq, cst], W=[pt])
                    e_ = act if half else dve
                    k.op(e_, CP(e_, h2Tv[:, half * 4:half * 4 + 4, :], pt[:].rearrange("p (a b) -> p a b", a=4)), R=[pt], W=[zs])
                pl = nf()
                for kc in range(KC):
                    k.op(pe, lambda h: h.matmul(pl[:, 0:NE], lhsT=h2Tv[:, kc, :], rhs=rw[:, kc, :], start=(kc == 0), stop=(kc == KC - 1)), R=[zs, rw], W=[pl])
                k.op(dve, lambda h: h.tensor_tensor(out=lg[:], in0=pl[:, 0:NE], in1=rb_bc[:], op=ALU.add), R=[pl, rb_bc], W=[lg])
                k.op(dve, lambda h: h.max(out=m8[:], in_=lg[:]), R=[lg], W=[m8])
                sel, ex, G_, pos, val, dest, oh, tmp = [rt[:, i, :] for i in range(8)]
                k.op(dve, lambda h: h.tensor_scalar(out=sel, in0=lg[:], scalar1=m8[:, 3:4], scalar2=None, op0=ALU.is_ge), R=[lg, m8], W=[rt])
                k.op(dve, lambda h: h.tensor_scalar(out=r1[:, 0:1], in0=m8[:, 0:1], scalar1=-1.0, scalar2=None, op0=ALU.mult), R=[m8], W=[r1])
                k.op(act, lambda h: h.activation(out=ex, in_=lg[:], func=AF.Exp, bias=r1[:, 0:1]), R=[lg, r1], W=[rt])
                k.op(dve, lambda h: h.tensor_tensor(out=ex, in0=ex, in1=sel, op=ALU.mult), R=[rt], W=[rt])
                k.op(dve, lambda h: h.tensor_reduce(out=r1[:, 1:2], in_=ex, axis=AX.X, op=ALU.add), R=[rt], W=[r1])
                k.op(dve, lambda h: h.reciprocal(out=r1[:, 2:3], in_=r1[:, 1:2]), R=[r1], W=[r1])
                k.op(dve, lambda h: h.tensor_scalar(out=G_, in0=ex, scalar1=r1[:, 2:3], scalar2=None, op0=ALU.mult), R=[rt, r1], W=[rt])
                k.op(pool, lambda h: h.tensor_copy(out=selb[:, it, :], in_=sel), R=[rt], W=[selb])
                pp = nf()
                for i2 in range(it):
                    k.op(pe, lambda h: h.matmul(pp[:, 0:NE], lhsT=ONES_b, rhs=selb[:, i2, :], start=(i2 == 0), stop=False), R=[cstb, selb], W=[pp])
                k.op(pe, lambda h: h.matmul(pp[:, 0:NE], lhsT=SUfull_b, rhs=selb[:, it, :], start=(it == 0), stop=True), R=[cstb, selb], W=[pp])
                k.op(dve, lambda h: h.tensor_copy(out=pos, in_=pp[:, 0:NE]), R=[pp], W=[rt])
                k.op(dve, lambda h: h.tensor_scalar(out=val, in0=pos, scalar1=float(CAP) - 0.5, scalar2=None, op0=ALU.is_lt), R=[rt], W=[rt])
                k.op(dve, lambda h: h.tensor_tensor(out=val, in0=val, in1=sel, op=ALU.mult), R=[rt], W=[rt])
                k.op(dve, lambda h: h.tensor_tensor(out=dest, in0=pos, in1=ec_bc[:], op=ALU.add), R=[rt, ec_bc], W=[rt])
                k.op(dve, lambda h: h.scalar_tensor_tensor(out=dest, in0=dest, scalar=-BIG, in1=val, op0=ALU.add, op1=ALU.mult), R=[rt], W=[rt])
                k.op(dve, lambda h: h.tensor_scalar(out=dest, in0=dest, scalar1=BIG, scalar2=None, op0=ALU.add), R=[rt], W=[rt])
                for kk in range(4):
                    k.op(dve, lambda h: h.tensor_scalar(out=oh, in0=lg[:], scalar1=m8[:, kk:kk + 1], scalar2=None, op0=ALU.is_equal), R=[lg, m8], W=[rt])
                    k.op(dve, lambda h: h.tensor_tensor(out=tmp, in0=oh, in1=dest, op=ALU.mult), R=[rt], W=[rt])
                    k.op(dve, lambda h: h.tensor_reduce(out=dkf[:, kk:kk + 1], in_=tmp, axis=AX.X, op=ALU.add), R=[rt], W=[dkf])
                    k.op(dve, lambda h: h.tensor_tensor(out=tmp, in0=oh, in1=G_, op=ALU.mult), R=[rt], W=[rt])
                    k.op(dve, lambda h: h.tensor_reduce(out=gk[:, it, kk:kk + 1], in_=tmp, axis=AX.X, op=ALU.add), R=[rt], W=[gk])
                k.op(dve, lambda h: h.tensor_copy(out=dki[:, it, :], in_=dkf[:]), R=[dkf], W=[dki])
                for kk in range(4):
                    k.dma(pool, lambda h: h.indirect_dma_start(out=xbuf[:, :], out_offset=bass.IndirectOffsetOnAxis(ap=dki[:, it, kk:kk + 1], axis=0),
                                                               in_=hb[:, :], in_offset=None, bounds_check=bc_s, oob_is_err=False),
                          d_sc, R=[hb, dki], W=[xbuf])
        k.barrier()

    if stop < 4:
        return
    with ExitStack() as es:
        w1 = [k.sb(f"w1_{i}", [P, KC, 2 * D], BF16, es) for i in range(2)]
        w2 = [k.sb(f"w2_{i}", [P, KC, D], BF16, es) for i in range(2)]
        d_w1 = [k.dsem(f"mw1{i}") for i in range(2)]
        d_w2 = [k.dsem(f"mw2{i}") for i in range(2)]
        b2bc = [k.sb(f"b2bc{i}", [P, D], F32, es) for i in range(2)]
        d_b2 = [k.dsem(f"mb2{i}") for i in range(2)]
        b1 = k.sb("b1", [P, NE, 16], F32, es)
        xe = [k.sb(f"xe{i}", [P, CT, D], BF16, es) for i in range(2)]
        d_xe = [k.dsem(f"xe{i}") for i in range(2)]
        xeT = k.sb("xeT", [P, KC, CAP], BF16, es)
        actT = k.sb("actT", [P, KC, CAP], BF16, es)
        NSC = -(-CAP // 384)
        CW = CAP // NSC
        g1 = [k.sb(f"g1_{i}", [P, CW], F32, es) for i in range(2)]
        sg = [k.sb(f"sg_{i}", [P, CW], F32, es) for i in range(2)]
        u1 = [k.sb(f"u1_{i}", [P, CW], F32, es) for i in range(2)]
        ysb = [k.sb(f"ysb{i}", [P, D], F32, es) for i in range(2)]
        d_y = [k.dsem(f"y{i}") for i in range(2)]
        psf = [k.ps(f"p3f{i}", [P, 512], F32, es) for i in range(6)]
        psb_t = [k.ps(f"p3b{i}", [P, 2, 4, P], BF16, es) for i in range(2)]
        rr = {"f": 0, "b": 0}

        def nf():
            rr["f"] += 1
            return psf[rr["f"] % 6]

        def nb():
            rr["b"] += 1
            return psb_t[(rr["b"] // 2) % 2], rr["b"] % 2
        k.dma(sp, lambda h: h.dma_start(out=b1[:], in_=moe_b1_fm), None, W=[b1])

        def load_w(e):
            z = e % 2
            k.dma(pool, lambda h: h.dma_start(out=w1[z][:], in_=moe_w1[e].rearrange("(k p) f -> p k f", p=P)), d_w1[z], W=[w1[z]])
            k.dma(pool, lambda h: h.dma_start(out=w2[z][:], in_=moe_w2[e].rearrange("(k p) f -> p k f", p=P)), d_w2[z], W=[w2[z]])
            k.dma(sp, lambda h: h.dma_start(out=b2bc[z][:], in_=moe_b2[e:e + 1, :].partition_broadcast(P)), d_b2[z], W=[b2bc[z]])
            k.dma(sp, lambda h: h.dma_start(out=xe[z][:], in_=xbuf[e * CAP:(e + 1) * CAP, :].rearrange("(c p) d -> p c d", p=P)), d_xe[z], R=[xbuf], W=[xe[z]])
        load_w(0)
        yi = 0
        zc = 0
        for e in range(NE):
            z = e % 2
            if e + 1 < NE:
                load_w(e + 1)
            for st in range(CT):
                for half in range(2):
                    pt, s = nb()
                    for j in range(4):
                        kc = half * 4 + j
                        k.op(pe, lambda h: h.transpose(out=pt[:, s, j, :], in_=xe[z][:, st, kc * P:(kc + 1) * P], identity=ident_b), R=[xe[z], cstb], W=[pt])
                    e_ = act if half else dve
                    k.op(e_, CP(e_, xeT[:, half * 4:half * 4 + 4, st * P:(st + 1) * P], pt[:, s, :, :]), R=[pt], W=[xeT])
            for fc in range(8):
                for sc_ in range(NSC):
                    zc += 1
                    zz = zc % 2
                    cs_ = slice(sc_ * CW, (sc_ + 1) * CW)
                    pg_, pu_ = nf(), nf()
                    for kc in range(KC):
                        k.op(pe, lambda h: h.matmul(pg_[:, 0:CW], lhsT=w1[z][:, kc, fc * P:(fc + 1) * P], rhs=xeT[:, kc, cs_], start=(kc == 0), stop=(kc == KC - 1)),
                             R=[w1[z], xeT], W=[pg_])
                    for kc in range(KC):
                        k.op(pe, lambda h: h.matmul(pu_[:, 0:CW], lhsT=w1[z][:, kc, D + fc * P:D + (fc + 1) * P], rhs=xeT[:, kc, cs_], start=(kc == 0), stop=(kc == KC - 1)),
                             R=[w1[z], xeT], W=[pu_])
                    k.op(dve, lambda h: h.tensor_scalar(out=g1[zz][:], in0=pg_[:, 0:CW], scalar1=b1[:, e, fc:fc + 1], scalar2=7.0, op0=ALU.add, op1=ALU.min),
                         R=[pg_, b1], W=[g1[zz]])
                    k.op(act, lambda h: h.activation(out=sg[zz][:], in_=g1[zz][:], func=AF.Sigmoid, scale=1.702), R=[g1[zz]], W=[sg[zz]])
                    k.op(dve, lambda h: h.tensor_scalar(out=u1[zz][:], in0=pu_[:, 0:CW], scalar1=b1[:, e, 8 + fc:9 + fc], scalar2=7.0, op0=ALU.add, op1=ALU.min),
                         R=[pu_, b1], W=[u1[zz]])
                    k.op(dve, lambda h: h.tensor_scalar(out=u1[zz][:], in0=u1[zz][:], scalar1=-7.0, scalar2=1.0, op0=ALU.max, op1=ALU.add), R=[u1[zz]], W=[u1[zz]])
                    k.op(dve, lambda h: h.tensor_tensor(out=g1[zz][:], in0=g1[zz][:], in1=sg[zz][:], op=ALU.mult), R=[g1[zz], sg[zz]], W=[g1[zz]])
                    k.op(dve, lambda h: h.tensor_tensor(out=actT[:, fc, cs_], in0=g1[zz][:], in1=u1[zz][:], op=ALU.mult), R=[g1[zz], u1[zz]], W=[actT])
            for st in range(CT):
                yi += 1
                Y = ysb[yi % 2]
                for half in range(2):
                    py = nf()
                    for fc in range(8):
                        k.op(pe, lambda h: h.matmul(py[:], lhsT=actT[:, fc, st * P:(st + 1) * P], rhs=w2[z][:, fc, half * 512:(half + 1) * 512],
                                                    start=(fc == 0), stop=(fc == 7)), R=[actT, w2[z]], W=[py])
                    hs_ = slice(half * 512, (half + 1) * 512)
                    k.op(dve, lambda h: h.tensor_tensor(out=Y[:, hs_], in0=py[:], in1=b2bc[z][:, hs_], op=ALU.add), R=[py, b2bc[z]], W=[Y])
                r0 = e * CAP + st * P
                k.dma(sp, lambda h: h.dma_start(out=ybuf[r0:r0 + P, :], in_=Y[:]), d_y[yi % 2], R=[Y], W=[ybuf])
        k.barrier()

    if stop < 5:
        return
    with ExitStack() as es:
        yk = [k.sb(f"yk{i}", [P, D], F32, es) for i in range(4)]
        fnw_bc = k.sb("fnw_bc", [P, D], F32, es)
        k.dma(sp, lambda h: h.dma_start(out=fnw_bc[:], in_=fnw.partition_broadcast(P)), None, W=[fnw_bc])
        d_yk = [k.dsem(f"yk{i}") for i in range(4)]
        x1t = k.sb("x1t", [P, D], F32, es)
        d_x1 = k.dsem("x1t")
        acc = k.sb("acc", [P, D], F32, es)
        junk = k.sb("junk3", [P, D], F32, es)
        ss3 = k.sb("ss3", [P, 1], F32, es)
        d_out = k.dsem("out")
        for i in range(4):
            k.op(pool, lambda h: h.memset(yk[i][:], 0.0), W=[yk[i]])
        for it in range(NTQ):
            k.dma(sp, lambda h: h.dma_start(out=x1t[:], in_=x1_d[it * P:(it + 1) * P, :]), d_x1, R=[x1_d], W=[x1t])
            for kk in range(4):
                k.dma(pool, lambda h: h.indirect_dma_start(out=yk[kk][:, :], out_offset=None, in_=ybuf[:, :],
                                                           in_offset=bass.IndirectOffsetOnAxis(ap=dki[:, it, kk:kk + 1], axis=0),
                                                           bounds_check=bc_s, oob_is_err=False), d_yk[kk], R=[ybuf, dki], W=[yk[kk]])
            k.op(dve, lambda h: h.tensor_scalar(out=acc[:], in0=yk[0][:], scalar1=gk[:, it, 0:1], scalar2=None, op0=ALU.mult), R=[yk[0], gk], W=[acc])
            for kk in range(1, 4):
                k.op(dve, lambda h: h.scalar_tensor_tensor(out=acc[:], in0=yk[kk][:], scalar=gk[:, it, kk:kk + 1], in1=acc[:], op0=ALU.mult, op1=ALU.add),
                     R=[yk[kk], gk, acc], W=[acc])
            k.op(pool, lambda h: h.tensor_tensor(out=acc[:], in0=acc[:], in1=vbc[:, 3, :], op=ALU.mult), R=[acc, vbc], W=[acc])
            k.op(pool, lambda h: h.tensor_tensor(out=acc[:], in0=acc[:], in1=x1t[:], op=ALU.add), R=[acc, x1t], W=[acc])
            k.op(act, lambda h: h.activation(out=junk[:], in_=acc[:], func=AF.Square), R=[acc], W=[junk])
            k.op(dve, lambda h: h.tensor_reduce(out=ss3[:], in_=junk[:], axis=AX.X, op=ALU.add), R=[junk], W=[ss3])
            rsqrt_(ss3, ss3[:], D * EPS)
            k.op(dve, lambda h: h.scalar_tensor_tensor(out=junk[:], in0=acc[:], scalar=ss3[:, 0:1], in1=fnw_bc[:], op0=ALU.mult, op1=ALU.mult), R=[acc, ss3, fnw_bc], W=[junk])
            k.op(pool, lambda h: h.tensor_scalar(out=junk[:], in0=junk[:], scalar1=float(D) ** 0.5, scalar2=None, op0=ALU.mult), R=[junk], W=[junk])
            k.dma(sp, lambda h: h.dma_start(out=out[it * P:(it + 1) * P, :], in_=junk[:]), d_out, R=[junk], W=[out])
        k.barrier()


def _fm(v, n):
    return np.ascontiguousarray(np.asarray(v, np.float32).reshape(n, P).T)


def prep(inp, TSEQ):
    g = {k_: np.asarray(v) for k_, v in inp.items()}
    w_in = g["w_in"][0]
    cuts = np.cumsum([3 * D, D, 16, 16, 3 * D, 2 * D])
    w_qkv = np.ascontiguousarray(w_in[:, :cuts[0]])
    w_z = np.ascontiguousarray(w_in[:, cuts[0]:cuts[1]])
    w_b = w_in[:, cuts[1]:cuts[2]]
    w_a = w_in[:, cuts[2]:cuts[3]]
    w_sc = np.ascontiguousarray(w_in[:, cuts[3]:cuts[4]])
    w_g = np.ascontiguousarray(w_in[:, cuts[4]:cuts[5]])
    w_bd = np.ascontiguousarray(np.concatenate([w_b[:, 0:8], w_a[:, 0:8], w_b[:, 8:16], w_a[:, 8:16]], axis=1))
    negA_dt = np.zeros((1, 64), np.float32)
    negA_dt[0, 0:16] = g["a_log"][0].reshape(16)
    negA_dt[0, 16:32] = g["dt_bias"][0].reshape(16)
    convq_fm = np.ascontiguousarray(g["conv_qkv_w"][0].T.reshape(24, P, 5).transpose(1, 0, 2))
    _, consts = _consts()
    shared = dict(ada_w=np.ascontiguousarray(g["ada_w"][0]), ada_b_fm=_fm(g["ada_b"][0], 48), n1_fm=_fm(g["norm1_w"][0], 8),
                  w_qkv=w_qkv, w_bd=w_bd, convq_fm=convq_fm, negA_dt=negA_dt, consts=np.ascontiguousarray(consts))
    TQ = TSEQ // 4
    NTQ = TQ // P
    CAP = P * int(np.ceil(CAPF * TQ / 8 / P))
    shared.update(
        w_z=w_z, w_sc=w_sc, w_g=w_g,
        convs_fm=np.ascontiguousarray(g["conv_sc_w"][0].T.reshape(8, P, 3).transpose(1, 0, 2)),
        onorm=np.ascontiguousarray(g["onorm_w"][0].reshape(1, P)),
        w_up_a=np.ascontiguousarray(g["w_up_a"][0]), w_out_sc=np.ascontiguousarray(g["w_out_sc"][0]), w_o=np.ascontiguousarray(g["w_o"][0]),
        n2_fm=_fm(g["norm2_w"][0], 8), router_w=np.ascontiguousarray(g["router_w"][0]),
        router_b=np.ascontiguousarray(g["router_b"][0].reshape(1, NE)),
        ecap=(np.arange(NE, dtype=np.float32) * CAP).reshape(1, NE),
        fnw=np.ascontiguousarray(g["final_norm_w"].reshape(1, D)),
        moe_w1=np.ascontiguousarray(g["moe_w1"][0]), moe_w2=np.ascontiguousarray(g["moe_w2"][0]),
        moe_b1_fm=np.ascontiguousarray(g["moe_b1"][0].reshape(NE, 16, P).transpose(2, 0, 1)),
        moe_b2=np.ascontiguousarray(g["moe_b2"][0]))
    maps = []
    pp = np.arange(P)[:, None]
    for c in range(8):
        b, q = c // 4, c % 4
        xb = np.ascontiguousarray(g["x"][b])
        m = dict(shared)
        m["xf"] = xb
        m["xr"] = np.ascontiguousarray(xb[::-1])
        m["c_fm"] = _fm(g["c"][b], 8)
        xo = np.zeros((TQ + 2, D), np.float32)
        lo, hi = q * TQ - 1, (q + 1) * TQ + 1
        xo[max(0, -lo):TQ + 2 - max(0, hi - TSEQ)] = xb[max(lo, 0):min(hi, TSEQ)]
        m["xo"] = xo
        m["hmask"] = np.repeat(np.array([[float(lo >= 0), float(hi <= TSEQ)]], np.float32), P, 0)
        tok = q * TQ + np.arange(NTQ)[None, :] * P + pp
        m["idx"] = np.ascontiguousarray(np.stack([tok, TSEQ - 1 - tok], axis=1).astype(np.int32))
        maps.append(m)
    return maps


_NC_CACHE = {}


def kernel(**inputs):
    TSEQ = int(np.asarray(inputs["x"]).shape[1])
    import os
    stop = int(os.environ.get("KSTOP", "99"))
    if TSEQ not in _NC_CACHE:
        _NC_CACHE[TSEQ] = build(TSEQ, stop=stop)
    maps = prep(inputs, TSEQ)
    res = run_bass_kernel_spmd(_NC_CACHE[TSEQ], maps, core_ids=list(range(8)))
    TQ = TSEQ // 4
    out = np.empty((2, TSEQ, D), np.float32)
    for c in range(8):
        out[c // 4, (c % 4) * TQ:(c % 4 + 1) * TQ] = res.results[c]["out"] if "out" in res.results[c] else 0.0
    return out
```
